# Optimizing a Trainium2 kernel written in Bass

```python
import math
import jax, jax.numpy as jnp
from jax import lax
import numpy as np

D_MODEL = 1024
BATCH = 8
SEQ = 4096
DEPTH = 2

HEAD_DIM = 64
SB_HEADS = 8
SB_WIDTH = SB_HEADS * HEAD_DIM
LRU_WIDTH = 512
LRU_BLOCKS = 8
LRU_BLOCK = LRU_WIDTH // LRU_BLOCKS
CONV_WIDTH = 4
LRU_C = 8.0
DIFF_HEADS = 8
DIFF_VDIM = 2 * HEAD_DIM
DIFF_QK = 2 * DIFF_HEADS * HEAD_DIM
DIFF_V = DIFF_HEADS * DIFF_VDIM
EVEN_IN = 3 * SB_WIDTH + 2 * LRU_WIDTH
ODD_IN = 2 * DIFF_QK + DIFF_V
N_BUCKETS = 32
MAX_EXACT = N_BUCKETS // 2
MAX_DISTANCE = 128
D_FF_DENSE = 2816
N_EXPERTS = 8
TOP_K = 2
D_FF_EXPERT = 3584
Q_BLOCK = 128
EPS = 1e-6

kernel_name = "hybrid_sb_rglru_diffattn_moe_adaln"


def rmsnorm(x, g):
    xf = x.astype(jnp.float32)
    n = xf * lax.rsqrt(jnp.mean(xf * xf, axis=-1, keepdims=True) + EPS)
    return (n * g.astype(jnp.float32)).astype(x.dtype)


def modulate(h, shift, scale):
    return h * (1.0 + scale[:, None, :]) + shift[:, None, :]


def _query_blocks(q):
    b, h, s, d = q.shape
    return jnp.moveaxis(q.reshape(b, h, s // Q_BLOCK, Q_BLOCK, d), 2, 0)


def _merge_blocks(o):
    nb, b, h, qb, d = o.shape
    return jnp.moveaxis(o, 0, 2).reshape(b, h, nb * qb, d)


def stick_breaking_attention(q, k, v):
    s_len = q.shape[2]
    scale = HEAD_DIM ** -0.5
    key_pos = jnp.arange(s_len, dtype=jnp.int32)

    def block(args):
        qb, blk = args
        z = jnp.einsum('bhqd,bhkd->bhqk', qb, k).astype(jnp.float32) * scale
        q_pos = blk * Q_BLOCK + jnp.arange(Q_BLOCK, dtype=jnp.int32)
        mask = key_pos[None, :] < q_pos[:, None]
        log_keep = jnp.where(mask, jax.nn.log_sigmoid(-z), 0.0)
        later = lax.cumsum(log_keep, axis=3, reverse=True) - log_keep
        w = jnp.where(mask, jnp.exp(jax.nn.log_sigmoid(z) + later), 0.0)
        return jnp.einsum('bhqk,bhkd->bhqd', w.astype(v.dtype), v)

    nb = s_len // Q_BLOCK
    out = lax.map(block, (_query_blocks(q), jnp.arange(nb, dtype=jnp.int32)))
    return _merge_blocks(out)


def rg_lru_branch(xb, gb, conv_w, conv_b, ga_w, ga_b, gx_w, gx_b, lam):
    b, s_len, c = xb.shape
    xc = lax.conv_general_dilated(
        xb, conv_w[:, None, :], window_strides=(1,), padding=[(CONV_WIDTH - 1, 0)],
        dimension_numbers=('NWC', 'WIO', 'NWC'), feature_group_count=c) + conv_b
    xg = xc.reshape(b, s_len, LRU_BLOCKS, LRU_BLOCK)
    r = jax.nn.sigmoid(jnp.einsum('bsgi,gij->bsgj', xg, ga_w).reshape(b, s_len, c) + ga_b)
    i = jax.nn.sigmoid(jnp.einsum('bsgi,gij->bsgj', xg, gx_w).reshape(b, s_len, c) + gx_b)
    log_a = LRU_C * r.astype(jnp.float32) * jax.nn.log_sigmoid(lam.astype(jnp.float32))
    a = jnp.exp(log_a)
    u = jnp.sqrt(-jnp.expm1(2.0 * log_a)) * (i * xc).astype(jnp.float32)

    def combine(left, right):
        a1, b1 = left
        a2, b2 = right
        return a1 * a2, a2 * b1 + b2

    _, h = lax.associative_scan(combine, (a, u), axis=1)
    return h.astype(xb.dtype) * jax.nn.gelu(gb)


def even_mixer(h, w_in, w_out, conv_w, conv_b, ga_w, ga_b, gx_w, gx_b, lam):
    b, s_len, _ = h.shape
    proj = h @ w_in
    q, k, v, xb, gb = jnp.split(
        proj, [SB_WIDTH, 2 * SB_WIDTH, 3 * SB_WIDTH, 3 * SB_WIDTH + LRU_WIDTH], axis=-1)
    heads = lambda t: t.reshape(b, s_len, SB_HEADS, HEAD_DIM).transpose(0, 2, 1, 3)
    ya = stick_breaking_attention(heads(q), heads(k), heads(v))
    ya = ya.transpose(0, 2, 1, 3).reshape(b, s_len, SB_WIDTH)
    yb = rg_lru_branch(xb, gb, conv_w, conv_b, ga_w, ga_b, gx_w, gx_b, lam)
    return jnp.concatenate([ya, yb], axis=-1) @ w_out


def t5_bucket(rel):
    n = jnp.maximum(rel, 0)
    nf = jnp.maximum(n, 1).astype(jnp.float32)
    large = MAX_EXACT + (jnp.log(nf / MAX_EXACT) / math.log(MAX_DISTANCE / MAX_EXACT)
                         * (N_BUCKETS - MAX_EXACT)).astype(jnp.int32)
    large = jnp.minimum(large, N_BUCKETS - 1)
    return jnp.where(n < MAX_EXACT, n, large)


def differential_attention(q, k, v, rel_bias, lam):
    b, _, s_len, _ = q.shape
    scale = HEAD_DIM ** -0.5
    key_pos = jnp.arange(s_len, dtype=jnp.int32)

    def block(args):
        qb, blk = args
        z = jnp.einsum('bhqd,bhkd->bhqk', qb, k).astype(jnp.float32) * scale
        z = z.reshape(b, DIFF_HEADS, 2, Q_BLOCK, s_len)
        q_pos = blk * Q_BLOCK + jnp.arange(Q_BLOCK, dtype=jnp.int32)
        rel = q_pos[:, None] - key_pos[None, :]
        bias = jnp.transpose(rel_bias[t5_bucket(rel)].astype(jnp.float32), (2, 0, 1))
        z = jnp.where(rel >= 0, z + bias[None, :, None], -jnp.inf)
        p = jax.nn.softmax(z, axis=-1)
        attn = p[:, :, 0] - lam * p[:, :, 1]
        return jnp.einsum('bhqk,bhkd->bhqd', attn.astype(v.dtype), v)

    nb = s_len // Q_BLOCK
    out = lax.map(block, (_query_blocks(q), jnp.arange(nb, dtype=jnp.int32)))
    return _merge_blocks(out)


def odd_mixer(h, w_in, w_out, rel_bias, lq1, lk1, lq2, lk2, subln, lambda_init):
    b, s_len, _ = h.shape
    proj = h @ w_in
    q, k, v = jnp.split(proj, [DIFF_QK, 2 * DIFF_QK], axis=-1)
    q = q.reshape(b, s_len, 2 * DIFF_HEADS, HEAD_DIM).transpose(0, 2, 1, 3)
    k = k.reshape(b, s_len, 2 * DIFF_HEADS, HEAD_DIM).transpose(0, 2, 1, 3)
    v = v.reshape(b, s_len, DIFF_HEADS, DIFF_VDIM).transpose(0, 2, 1, 3)
    lam = (jnp.exp(jnp.sum(lq1.astype(jnp.float32) * lk1.astype(jnp.float32)))
           - jnp.exp(jnp.sum(lq2.astype(jnp.float32) * lk2.astype(jnp.float32)))
           + lambda_init)
    o = differential_attention(q, k, v, rel_bias, lam)
    o = rmsnorm(o, subln) * (1.0 - lambda_init)
    return o.transpose(0, 2, 1, 3).reshape(b, s_len, DIFF_V) @ w_out


def swiglu(h, w_gate, w_up, w_down):
    return (jax.nn.silu(h @ w_gate) * (h @ w_up)) @ w_down


def moe_ffn(h, router_w, router_b, wg, wu, wd):
    logits = (jnp.einsum('bsd,de->bse', h, router_w) + router_b).astype(jnp.float32)
    top_vals, top_idx = lax.top_k(logits, TOP_K)
    top_w = jax.nn.softmax(top_vals, axis=-1)
    gates = jnp.einsum('bsk,bske->bse', top_w,
                       jax.nn.one_hot(top_idx, N_EXPERTS, dtype=jnp.float32))
    y = jnp.zeros_like(h)
    for e in range(N_EXPERTS):
        y = y + gates[..., e:e + 1].astype(h.dtype) * swiglu(h, wg[e], wu[e], wd[e])
    return y


def setup_inputs(seed: int = 0) -> dict:
    key = jax.random.key(seed)
    ks = jax.random.split(key, 32)
    f32 = jnp.float32
    ne = (DEPTH + 1) // 2
    no = DEPTH // 2
    nrm = lambda k, shape, s: jax.random.normal(k, shape, f32) * s
    u = jax.random.uniform(ks[16], (ne, LRU_WIDTH), f32, 0.9, 0.999)
    root = u ** (1.0 / LRU_C)
    return {
        "x": nrm(ks[0], (BATCH, SEQ, D_MODEL), 1.0),
        "c": nrm(ks[1], (BATCH, D_MODEL), 1.0),
        "rel_bias": nrm(ks[2], (N_BUCKETS, DIFF_HEADS), 0.5),
        "ada_w": nrm(ks[3], (DEPTH, D_MODEL, 6 * D_MODEL), D_MODEL ** -0.5),
        "ada_b": nrm(ks[4], (DEPTH, 6 * D_MODEL), 0.02),
        "ln_mix": 1.0 + nrm(ks[5], (DEPTH, D_MODEL), 0.1),
        "ln_ffn": 1.0 + nrm(ks[6], (DEPTH, D_MODEL), 0.1),
        "ln_final": 1.0 + nrm(ks[7], (D_MODEL,), 0.1),
        "even_w_in": nrm(ks[8], (ne, D_MODEL, EVEN_IN), D_MODEL ** -0.5),
        "even_w_out": nrm(ks[9], (ne, SB_WIDTH + LRU_WIDTH, D_MODEL), (SB_WIDTH + LRU_WIDTH) ** -0.5),
        "lru_conv_w": nrm(ks[10], (ne, CONV_WIDTH, LRU_WIDTH), CONV_WIDTH ** -0.5),
        "lru_conv_b": nrm(ks[11], (ne, LRU_WIDTH), 0.02),
        "lru_gate_a_w": nrm(ks[12], (ne, LRU_BLOCKS, LRU_BLOCK, LRU_BLOCK), LRU_BLOCK ** -0.5),
        "lru_gate_a_b": nrm(ks[13], (ne, LRU_WIDTH), 0.02),
        "lru_gate_x_w": nrm(ks[14], (ne, LRU_BLOCKS, LRU_BLOCK, LRU_BLOCK), LRU_BLOCK ** -0.5),
        "lru_gate_x_b": nrm(ks[15], (ne, LRU_WIDTH), 0.02),
        "lru_lambda": jnp.log(root) - jnp.log1p(-root),
        "ffn_w_gate": nrm(ks[17], (ne, D_MODEL, D_FF_DENSE), D_MODEL ** -0.5),
        "ffn_w_up": nrm(ks[18], (ne, D_MODEL, D_FF_DENSE), D_MODEL ** -0.5),
        "ffn_w_down": nrm(ks[19], (ne, D_FF_DENSE, D_MODEL), D_FF_DENSE ** -0.5),
        "odd_w_in": nrm(ks[20], (no, D_MODEL, ODD_IN), D_MODEL ** -0.5),
        "odd_w_out": nrm(ks[21], (no, DIFF_V, D_MODEL), DIFF_V ** -0.5),
        "diff_lambda_q1": nrm(ks[22], (no, HEAD_DIM), 0.1),
        "diff_lambda_k1": nrm(ks[23], (no, HEAD_DIM), 0.1),
        "diff_lambda_q2": nrm(ks[24], (no, HEAD_DIM), 0.1),
        "diff_lambda_k2": nrm(ks[25], (no, HEAD_DIM), 0.1),
        "diff_subln": 1.0 + nrm(ks[26], (no, DIFF_VDIM), 0.1),
        "router_w": nrm(ks[27], (no, D_MODEL, N_EXPERTS), D_MODEL ** -0.5),
        "router_b": nrm(ks[28], (no, N_EXPERTS), 0.01),
        "moe_w_gate": nrm(ks[29], (no, N_EXPERTS, D_MODEL, D_FF_EXPERT), D_MODEL ** -0.5),
        "moe_w_up": nrm(ks[30], (no, N_EXPERTS, D_MODEL, D_FF_EXPERT), D_MODEL ** -0.5),
        "moe_w_down": nrm(ks[31], (no, N_EXPERTS, D_FF_EXPERT, D_MODEL), D_FF_EXPERT ** -0.5),
    }


def reference(x, c, rel_bias, ada_w, ada_b, ln_mix, ln_ffn, ln_final,
              even_w_in, even_w_out, lru_conv_w, lru_conv_b, lru_gate_a_w, lru_gate_a_b,
              lru_gate_x_w, lru_gate_x_b, lru_lambda, ffn_w_gate, ffn_w_up, ffn_w_down,
              odd_w_in, odd_w_out, diff_lambda_q1, diff_lambda_k1, diff_lambda_q2,
              diff_lambda_k2, diff_subln, router_w, router_b, moe_w_gate, moe_w_up, moe_w_down):
    cond = jax.nn.silu(c)
    for layer in range(DEPTH):
        i = layer // 2
        mod = cond @ ada_w[layer] + ada_b[layer]
        sh1, sc1, g1, sh2, sc2, g2 = jnp.split(mod, 6, axis=-1)
        h = modulate(rmsnorm(x, ln_mix[layer]), sh1, sc1)
        if layer % 2 == 0:
            mix = even_mixer(h, even_w_in[i], even_w_out[i], lru_conv_w[i], lru_conv_b[i],
                             lru_gate_a_w[i], lru_gate_a_b[i], lru_gate_x_w[i], lru_gate_x_b[i],
                             lru_lambda[i])
        else:
            lambda_init = 0.8 - 0.6 * math.exp(-0.3 * layer)
            mix = odd_mixer(h, odd_w_in[i], odd_w_out[i], rel_bias, diff_lambda_q1[i],
                            diff_lambda_k1[i], diff_lambda_q2[i], diff_lambda_k2[i],
                            diff_subln[i], lambda_init)
        x = x + g1[:, None, :] * mix
        h = modulate(rmsnorm(x, ln_ffn[layer]), sh2, sc2)
        if layer % 2 == 0:
            ffn = swiglu(h, ffn_w_gate[i], ffn_w_up[i], ffn_w_down[i])
        else:
            ffn = moe_ffn(h, router_w[i], router_b[i], moe_w_gate[i], moe_w_up[i], moe_w_down[i])
        x = x + g2[:, None, :] * ffn
    return rmsnorm(x, ln_final)
```

```python
import math
from contextlib import ExitStack
import numpy as np
import concourse.bass as bass
import concourse.mybir as mybir
from concourse.bass_utils import run_bass_kernel_spmd

F32 = mybir.dt.float32
BF16 = mybir.dt.bfloat16
AF = mybir.ActivationFunctionType
ALU = mybir.AluOpType
AX = mybir.AxisListType

S = 4096
D = 1024
NT = S // 128
EPS = 1e-6
LAMBDA_INIT = 0.8 - 0.6 * math.exp(-0.3 * 1)


class Buf:
    __slots__ = ("w", "r")

    def __init__(self):
        self.w = {}
        self.r = {}


def bufs(n):
    return [Buf() for _ in range(n)]


class Prog:
    CE = ("scalar", "vector", "gpsimd", "tensor")
    DQ = {"sync": 8, "scalar": 3, "gpsimd": 8}

    def __init__(self, nc, ctx):
        self.nc = nc
        self.h = {e: getattr(nc, e) for e in ("sync", "scalar", "vector", "gpsimd", "tensor")}
        self.semh = {}
        self.cnt = {}
        for e in self.CE:
            k = "e_" + e
            self.semh[k] = ctx.enter_context(nc.semaphore(k))
            self.cnt[k] = 0
        self.drr = {}
        for q, n in self.DQ.items():
            self.drr[q] = 0
            for i in range(n):
                k = "d_%s_%d" % (q, i)
                self.semh[k] = ctx.enter_context(nc.semaphore(k))
                self.cnt[k] = 0
        self.seen = {e: {} for e in self.h}
        self.ninst = 0

    def _waits(self, eng, r, w, extra=()):
        need = {}

        def add(d):
            for k, v in d.items():
                if need.get(k, 0) < v:
                    need[k] = v

        for b in r:
            add(b.w)
        for b in w:
            add(b.w)
            add(b.r)
        for k, v in extra:
            if need.get(k, 0) < v:
                need[k] = v
        seen = self.seen[eng]
        h = self.h[eng]
        for k, v in need.items():
            if seen.get(k, 0) < v:
                h.wait_ge(self.semh[k], v)
                seen[k] = v
                self.ninst += 1

    def _record(self, tok, r, w):
        k, v = tok
        for b in r:
            b.r[k] = v
        for b in w:
            if b.r:
                b.w = {k: v}
                b.r = {}
            else:
                b.w[k] = v

    def op(self, eng, fn, r=(), w=()):
        self._waits(eng, r, w)
        ins = fn(self.h[eng])
        k = "e_" + eng
        self.cnt[k] += 1
        ins.then_inc(self.semh[k], 1)
        self.ninst += 1
        self._record((k, self.cnt[k]), r, w)

    def dma(self, q, out, in_, r=(), w=(), **kw):
        i = self.drr[q]
        self.drr[q] = (i + 1) % self.DQ[q]
        k = "d_%s_%d" % (q, i)
        self._waits(q, r, w, extra=((k, self.cnt[k]),) if self.cnt[k] else ())
        ins = self.h[q].dma_start(out=out, in_=in_, **kw)
        self.cnt[k] += 16
        ins.then_inc(self.semh[k], 16)
        self.ninst += 1
        self._record((k, self.cnt[k]), r, w)

    def barrier(self, engines=("sync", "scalar", "vector", "gpsimd", "tensor")):
        for e in engines:
            seen = self.seen[e]
            for k, v in self.cnt.items():
                if v and seen.get(k, 0) < v:
                    self.h[e].wait_ge(self.semh[k], v)
                    seen[k] = v


class Ring:
    def __init__(self, tiles):
        self.t = tiles
        self.b = bufs(len(tiles))
        self.i = 0

    def next(self):
        i = self.i
        self.i = (i + 1) % len(self.t)
        return self.t[i], self.b[i]


NEED = {"even_w_in": 1, "lru_vec": 2, "lru_ga": 2, "lru_gx": 2, "lmask": 3, "even_w_out": 4, "ffn_wg": 5, "ffn_wu": 5,
        "ffn_wd": 5, "odd_w_in": 6, "rel_bias": 7, "dl": 7, "subln": 7, "bk": 7, "negm": 7, "odd_w_out": 8,
        "router_w": 9, "router_b": 9, "moe_wg": 9, "moe_wu": 9, "moe_wd": 9}


def build(dump=(), upto=99, moe_groups=4, moe_nexp=8, skip=(), lo_ph=1, RT=99):
    nc = bass.Bass("TRN2", target_bir_lowering=False)
    ctx = ExitStack()
    P = Prog(nc, ctx)
    P.in_names = []

    def din(name, shape, dt=F32):
        if NEED.get(name, 0) > upto or (0 < NEED.get(name, 0) < lo_ph and NEED.get(name, 0) != 9) or (moe_nexp == 0 and name.startswith("moe_w")):
            return None
        P.in_names.append(name)
        return nc.dram_tensor(name, list(shape), dt, kind="ExternalInput").ap()

    def dscr(name, shape, dt):
        return nc.dram_tensor(name, list(shape), dt, kind=("ExternalOutput" if name in dump else "Internal")).ap()

    x_in = din("x", [S, D])
    cT = din("cT", [128, 8])
    rel_bias = din("rel_bias", [1, 256])
    ada_w = din("ada_w", [2, D, 6 * D])
    ada_b = din("ada_b", [1, 12 * D])
    ln_all = din("ln_all", [1, 5 * D])
    even_w_in = din("even_w_in", [D, 2560])
    even_w_out = din("even_w_out", [D, D])
    lru_vec = din("lru_vec", [128, 4, 8])
    lru_ga = din("lru_ga", [8, 64, 64])
    lru_gx = din("lru_gx", [8, 64, 64])
    ffn_wg = din("ffn_wg", [1, D, 2816])
    ffn_wu = din("ffn_wu", [1, D, 2816])
    ffn_wd = din("ffn_wd", [1, 2816, D])
    odd_w_in = din("odd_w_in", [D, 3072])
    odd_w_out = din("odd_w_out", [D, D])
    dl = din("dl", [1, 256])
    subln = din("subln", [1, 128])
    router_w = din("router_w", [D, 8])
    router_b = din("router_b", [1, 8])
    moe_wg = din("moe_wg", [8, D, 3584])
    moe_wu = din("moe_wu", [8, D, 3584])
    moe_wd = din("moe_wd", [8, 3584, D])
    ident_d = din("ident", [128, 128])
    lmask_d = din("lmask", [128, 128])
    bk_d = din("bk", [128, 256])
    negm_d = din("negm", [128, 128])
    out_d = nc.dram_tensor("out", [S, D], F32, kind="ExternalOutput").ap()

    mod_d = dscr("mod_d", [1, 12 * D], F32)
    qk_d = dscr("qk_d", [16, 128, S], BF16)
    v_d = dscr("v_d", [S, 1024], BF16)
    xg_d = dscr("xg_d", [8, 128, S], F32)
    yT_d = dscr("yT_d", [8, 128, S], BF16)
    gates_dd = dscr("gates_d", [128, NT, 8], F32) if "gates_d" in dump else None
    lg_dd = dscr("lg_d", [128, NT, 32], F32) if "lg_d" in dump else None
    h32_dd = dscr("h32_d", [128, 8, 128], F32) if "lg_d" in dump else None
    xs_d = [dscr("xs%d_d" % i, [S, D], F32) for i in range(3)]

    B_mod = Buf()
    B_qk = bufs(16)
    B_v = bufs(NT)
    B_xg = bufs(8)
    B_yT = bufs(8)
    B_xs = [bufs(NT) for _ in range(3)]
    B_out = bufs(NT)

    def sb(c, name, shape, dt=F32):
        return c.enter_context(nc.sbuf_tensor(name, list(shape), dt))

    def ps(c, name, shape, dt=F32):
        return c.enter_context(nc.psum_tensor(name, list(shape), dt))

    ident = sb(ctx, "ident_sb", [128, 128])
    B_ident = Buf()
    P.dma("sync", ident[:], ident_d[:, :], w=[B_ident])
    ones1 = sb(ctx, "ones1", [1, 128])
    B_ones1 = Buf()
    P.op("vector", lambda h: h.memset(ones1[:], 1.0), w=[B_ones1])

    with ExitStack() as c0:
        cond = sb(c0, "cond", [128, 8])
        B_cond = Buf()
        P.dma("sync", cond[:], cT[:, :], w=[B_cond])
        P.op("scalar", lambda h: h.activation(out=cond[:], in_=cond[:], func=AF.Silu), r=[B_cond], w=[B_cond])
        modrow = sb(c0, "modrow", [1, 12 * D])
        B_modrow = Buf()
        brow = sb(c0, "brow", [1, 12 * D])
        B_brow = Buf()
        P.dma("sync", brow[:], ada_b[:, :], w=[B_brow])
        lnrow = sb(c0, "lnrow", [1, 5 * D])
        B_lnrow = Buf()
        P.dma("sync", lnrow[:], ln_all[:, :], w=[B_lnrow])
        wring = Ring([sb(c0, "adaw%d" % i, [128, 8, 512]) for i in range(2)])
        pring = Ring([ps(c0, "adaps%d" % i, [128, 512])[0:1, :] for i in range(2)])
        for l in range(2):
            wv = ada_w[l].rearrange("(kc p) n -> p kc n", p=128)
            for j in range(12):
                wt, wb = wring.next()
                P.dma("sync" if j % 2 == 0 else "scalar", wt[:], wv[:, :, j * 512:(j + 1) * 512], w=[wb])
                pt, pb = pring.next()

                def fn(h, wt=wt, pt=pt):
                    for kc in range(8):
                        ins = h.matmul(pt[:], cond[:, kc:kc + 1], wt[:, kc, :], start=(kc == 0), stop=(kc == 7))
                    return ins
                P.op("tensor", fn, r=[B_cond, wb], w=[pb])
                o = l * 6144 + j * 512
                P.op("vector", lambda h, pt=pt, o=o: h.tensor_tensor(modrow[:, o:o + 512], pt[:], brow[:, o:o + 512], ALU.add),
                     r=[pb, B_brow], w=[B_modrow])
        for l in range(2):
            for s_, (sc_off, ln_off) in enumerate(((1, 2 * l), (4, 2 * l + 1))):
                o = l * 6144 + sc_off * 1024
                lo = ln_off * 1024
                P.op("vector", lambda h, o=o, lo=lo: h.scalar_tensor_tensor(
                    modrow[:, o:o + 1024], modrow[:, o:o + 1024], 1.0, lnrow[:, lo:lo + 1024], ALU.add, ALU.mult),
                    r=[B_modrow, B_lnrow], w=[B_modrow])
        P.dma("sync", mod_d[:, :], modrow[:], r=[B_modrow], w=[B_mod])
    P.barrier()

    def load_bc(c, name, off, q="sync", n=1024, src=None, rb=None):
        t = sb(c, name, [128, n])
        b = Buf()
        s_ = (mod_d if src is None else src)[0:1, off:off + n]
        P.dma(q, t[:], s_.to_broadcast([128, n]), r=[B_mod if rb is None else rb], w=[b])
        return t, b

    class NormCtx:
        def __init__(self, c, tag, gm_off, sh_off, want32=False):
            self.gm, self.b_gm = load_bc(c, tag + "gm", gm_off)
            self.sh, self.b_sh = load_bc(c, tag + "sh", sh_off, q="scalar")
            self.xr = Ring([sb(c, tag + "xt%d" % i, [128, D]) for i in range(2)])
            self.hr = Ring([sb(c, tag + "ht%d" % i, [128, D]) for i in range(2)])
            self.jr = Ring([sb(c, tag + "junk%d" % i, [128, D]) for i in range(1)])
            self.sr = Ring([sb(c, tag + "st%d" % i, [128, 4]) for i in range(2)])
            self.pr = Ring([ps(c, tag + "tp%d" % i, [128, 512]) for i in range(2)])
            self.want32 = want32
            self.k = 0

        def run(self, xsrc_ap, xsrc_bufs, hT, hT_buf, col0, hT32=None, hT32_buf=None, keep_x=None):
            if keep_x is not None:
                xt, xb = keep_x
            else:
                xt, xb = self.xr.next()
            P.dma("sync", xt[:], xsrc_ap, r=xsrc_bufs, w=[xb])
            jt, jb = self.jr.next()
            st, stb = self.sr.next()
            P.op("vector", lambda h: h.memset(st[:], 0.0), w=[stb])
            P.op("scalar", lambda h: h.activation(out=jt[:], in_=xt[:], func=AF.Square, accum_out=st[:, 0:1]),
                 r=[xb], w=[jb, stb])
            P.op("scalar", lambda h: h.activation(out=st[:, 1:2], in_=st[:, 0:1], func=AF.Sqrt, scale=1.0 / D, bias=EPS_AP[:, 0:1]),
                 r=[stb, B_eps], w=[stb])
            P.op("vector", lambda h: h.reciprocal(st[:, 2:3], st[:, 1:2]), r=[stb], w=[stb])
            ht, hb = self.hr.next()
            P.op("vector", lambda h: h.scalar_tensor_tensor(ht[:], xt[:], st[:, 2:3], self.gm[:], ALU.mult, ALU.mult),
                 r=[xb, stb, self.b_gm], w=[hb])
            P.op("vector", lambda h: h.tensor_tensor(ht[:], ht[:], self.sh[:], ALU.add), r=[hb, self.b_sh], w=[hb])
            for half in range(2):
                pt, pb = self.pr.next()

                def fn(h, pt=pt, half=half):
                    for j in range(4):
                        kc = half * 4 + j
                        ins = h.transpose(pt[:, j * 128:(j + 1) * 128], ht[:, kc * 128:(kc + 1) * 128], ident[:])
                    return ins
                P.op("tensor", fn, r=[hb, B_ident], w=[pb])
                dst = hT[:, half * 4:(half + 1) * 4, col0:col0 + 128]
                src = pt[:].rearrange("p (j t) -> p j t", j=4)
                eng = "scalar" if (self.k % 2 == 0) else "vector"
                self.k += 1
                if hT32 is not None:
                    dst2 = hT32[:, half * 4:(half + 1) * 4, :]
                    P.op("vector", lambda h: h.tensor_copy(dst2, src), r=[pb], w=[hT32_buf])
                    P.op("scalar", lambda h: h.copy(dst, dst2), r=[hT32_buf], w=[hT_buf])
                elif eng == "scalar":
                    P.op("scalar", lambda h: h.copy(dst, src), r=[pb], w=[hT_buf])
                else:
                    P.op("vector", lambda h: h.tensor_copy(dst, src), r=[pb], w=[hT_buf])
            return xt, xb

    EPS_AP = sb(ctx, "eps_ap", [128, 1])
    B_eps = Buf()
    P.op("vector", lambda h: h.memset(EPS_AP[:], EPS), w=[B_eps])

    def proj_phase(tag, xsrc, xbufs, w_in, gm_off, sh_off, specs):
        with ExitStack() as c:
            hT = sb(c, tag + "hT", [128, 8, S], BF16)
            B_hT = bufs(NT)
            with ExitStack() as c1:
                ncx = NormCtx(c1, tag + "n", gm_off, sh_off)
                for t in range(NT):
                    ncx.run(xsrc[t * 128:(t + 1) * 128, :], [xbufs[t]] if xbufs else [], hT, B_hT[t], t * 128)
            P.barrier()
            wv = w_in.rearrange("(kc p) n -> p kc n", p=128)
            wr = Ring([sb(c, tag + "w%d" % i, [128, 8, 512], BF16) for i in range(2)])
            pr = Ring([ps(c, tag + "pp%d" % i, [128, 512]) for i in range(4)])
            stF32 = Ring([sb(c, tag + "sf%d" % i, [128, S], F32) for i in range(2)])
            stF16 = Ring([sb(c, tag + "sh%d" % i, [128, S], BF16) for i in range(2)])
            stT = Ring([sb(c, tag + "stT%d" % i, [128, 512], BF16) for i in range(3)])
            k = 0
            for (col0, ncols, mode, dst, dbufs, dt, chunk0) in specs:
                for c512 in range(ncols // 512):
                    wt, wb = wr.next()
                    P.dma("gpsimd", wt[:], wv[:, :, col0 + c512 * 512: col0 + (c512 + 1) * 512], w=[wb])
                    if mode == "F":
                        for fc in range(4):
                            stg, sgb = (stF32 if dt == F32 else stF16).next()
                            for nt in range(8):
                                pt, pb = pr.next()

                                def fn(h, pt=pt, wt=wt, fc=fc, nt=nt):
                                    for kc in range(8):
                                        ins = h.matmul(pt[:], wt[:, kc, fc * 128:(fc + 1) * 128],
                                                       hT[:, kc, nt * 512:(nt + 1) * 512], start=(kc == 0), stop=(kc == 7))
                                    return ins
                                P.op("tensor", fn, r=[wb] + B_hT[nt * 4:(nt + 1) * 4], w=[pb])
                                k += 1
                                if k % 2 == 0:
                                    P.op("scalar", lambda h, pt=pt, stg=stg, nt=nt: h.copy(stg[:, nt * 512:(nt + 1) * 512], pt[:]),
                                         r=[pb], w=[sgb])
                                else:
                                    P.op("vector", lambda h, pt=pt, stg=stg, nt=nt: h.tensor_copy(stg[:, nt * 512:(nt + 1) * 512], pt[:]),
                                         r=[pb], w=[sgb])
                            ch = chunk0 + c512 * 4 + fc
                            P.dma("sync", dst[ch, :, :], stg[:], r=[sgb], w=[dbufs[ch]])
                    else:
                        for t in range(NT):
                            pt, pb = pr.next()

                            def fn(h, pt=pt, wt=wt, t=t):
                                for kc in range(8):
                                    ins = h.matmul(pt[:], hT[:, kc, t * 128:(t + 1) * 128], wt[:, kc, :],
                                                   start=(kc == 0), stop=(kc == 7))
                                return ins
                            P.op("tensor", fn, r=[wb, B_hT[t]], w=[pb])
                            stg, sgb = stT.next()
                            k += 1
                            if k % 2 == 0:
                                P.op("scalar", lambda h, pt=pt, stg=stg: h.copy(stg[:], pt[:]), r=[pb], w=[sgb])
                            else:
                                P.op("vector", lambda h, pt=pt, stg=stg: h.tensor_copy(stg[:], pt[:]), r=[pb], w=[sgb])
                            P.dma("sync", dst[t * 128:(t + 1) * 128, c512 * 512:(c512 + 1) * 512], stg[:], r=[sgb], w=[dbufs[t]])
        P.barrier()

    def outproj_phase(tag, xsrc, xbufs, xdst, xdbufs, w_out, g_off):
        with ExitStack() as c:
            gbc, b_g = load_bc(c, tag + "g", g_off)
            wv = w_out.rearrange("(kc p) n -> p kc n", p=128)
            wt = sb(c, tag + "w", [128, 8, D], BF16)
            B_w = Buf()
            P.dma("gpsimd", wt[:, :, 0:512], wv[:, :, 0:512], w=[B_w])
            P.dma("gpsimd", wt[:, :, 512:1024], wv[:, :, 512:1024], w=[B_w])
            yr = Ring([sb(c, tag + "y%d" % i, [128, 8, 512], BF16) for i in range(2)])
            xr = Ring([sb(c, tag + "x%d" % i, [128, D]) for i in range(3)])
            pr = Ring([ps(c, tag + "p%d" % i, [128, 512]) for i in range(4)])
            tr = Ring([sb(c, tag + "t%d" % i, [128, 512]) for i in range(2)])
            for g4 in range(8):
                yt, yb = yr.next()
                P.dma("sync", yt[:], yT_d[:, :, g4 * 512:(g4 + 1) * 512].rearrange("k p t -> p k t"), r=B_yT, w=[yb])
                for tt in range(4):
                    t = g4 * 4 + tt
                    xt, xb = xr.next()
                    P.dma("scalar", xt[:], xsrc[t * 128:(t + 1) * 128, :], r=[xbufs[t]] if xbufs else [], w=[xb])
                    for half in range(2):
                        pt, pb = pr.next()

                        def fn(h, pt=pt, yt=yt, tt=tt, half=half):
                            for kc in range(8):
                                ins = h.matmul(pt[:], yt[:, kc, tt * 128:(tt + 1) * 128], wt[:, kc, half * 512:(half + 1) * 512],
                                               start=(kc == 0), stop=(kc == 7))
                            return ins
                        P.op("tensor", fn, r=[yb, B_w], w=[pb])
                        sl = slice(half * 512, (half + 1) * 512)
                        tt_, ttb = tr.next()
                        P.op("vector", lambda h, pt=pt, xt=xt, sl=sl: h.tensor_tensor(tt_[:], pt[:], gbc[:, sl], ALU.mult),
                             r=[pb, b_g], w=[ttb])
                        P.op("gpsimd", lambda h, pt=pt, xt=xt, sl=sl: h.tensor_tensor(xt[:, sl], xt[:, sl], tt_[:], ALU.add),
                             r=[ttb, xb], w=[xb])
                    P.dma("sync", xdst[t * 128:(t + 1) * 128, :], xt[:], r=[xb], w=[xdbufs[t]])
        P.barrier()

    def ffn_phase(tag, xsrc, xbufs, xdst, xdbufs, wg, wu, wd, n_exp, dff, gm_off, sh_off, g_off, final=False):
        G = 1024
        ftiles = []
        f0 = 0
        while f0 < dff:
            wdt = min(512, dff - f0)
            ftiles.append((f0, wdt))
            f0 += wdt
        subgroups = [ftiles[i:i + 2] for i in range(0, len(ftiles), 2)]
        with ExitStack() as c:
            gbc, b_g = load_bc(c, tag + "g", g_off)
            if final:
                lnf, b_lnf = load_bc(c, tag + "lnf", 4 * D, src=ln_all, rb=Buf())
            ncx = NormCtx(c, tag + "n", gm_off, sh_off)
            hT = sb(c, tag + "hT", [128, 8, G], BF16)
            ysum = sb(c, tag + "ysum", [128, 8, D])
            B_ys = bufs(8)
            actT = sb(c, tag + "actT", [128, 8, G], BF16)
            wgr = Ring([sb(c, tag + "wg%d" % i, [128, 8, 512], BF16) for i in range(2)])
            wur = Ring([sb(c, tag + "wu%d" % i, [128, 8, 512], BF16) for i in range(2)])
            wdr = Ring([sb(c, tag + "wd%d" % i, [128, 8, D], BF16) for i in range(2)])
            sgr = Ring([sb(c, tag + "sg%d" % i, [128, 512]) for i in range(3)])
            psg = Ring([ps(c, tag + "pg%d" % i, [128, 512]) for i in range(2)])
            psu = Ring([ps(c, tag + "pu%d" % i, [128, 512]) for i in range(2)])
            psd = Ring([ps(c, tag + "pd%d" % i, [128, 512]) for i in range(2)])
            if n_exp > 1:
                gates = sb(c, tag + "gates", [128, 8, 8])
                hT32r = Ring([sb(c, tag + "h32_%d" % i, [128, 8, 128]) for i in range(2)])
                rw = sb(c, tag + "rw", [128, 8, 8])
                B_rw = Buf()
                P.dma("sync", rw[:], router_w.rearrange("(kc p) e -> p kc e", p=128), w=[B_rw])
                rbb, b_rbb = load_bc(c, tag + "rb", 0, n=8, src=router_b, rb=Buf())
                lgr = Ring([sb(c, tag + "lg%d" % i, [128, 32]) for i in range(2)])
            B_hT = bufs(8)
            B_act = Buf()
            B_gates = bufs(8)
            for g in range(min(S // G, moe_groups if n_exp > 1 else 99)):
                for tt in range(8):
                    t = g * 8 + tt
                    if n_exp > 1 and 'h32' not in skip:
                        h32, h32b = hT32r.next()
                    else:
                        h32, h32b = None, None
                    ncx.run(xsrc[t * 128:(t + 1) * 128, :], [xbufs[t]] if xbufs else [], hT, B_hT[tt], tt * 128,
                            hT32=h32, hT32_buf=h32b)
                    if n_exp > 1 and 'router' not in skip:
                        pt, pb = psd.next()
                        _rt = [0]

                        def POP(*a, **k):
                            _rt[0] += 1
                            if _rt[0] <= RT:
                                P.op(*a, **k)

                        def fn(h, pt=pt, h32=h32):
                            for kc in range(8):
                                ins = h.matmul(pt[:, 0:8], h32[:, kc, :], rw[:, kc, :], start=(kc == 0), stop=(kc == 7))
                            return ins
                        POP("tensor", fn, r=[h32b, B_rw], w=[pb])
                        lg, lgb = lgr.next()
                        POP("vector", lambda h: h.tensor_tensor(lg[:, 0:8], pt[:, 0:8], rbb[:], ALU.add), r=[pb, b_rbb], w=[lgb])
                        POP("vector", lambda h: h.max(lg[:, 8:16], lg[:, 0:8]), r=[lgb], w=[lgb])
                        POP("vector", lambda h: h.tensor_scalar(lg[:, 24:25], lg[:, 8:9], -1.0, None, ALU.mult), r=[lgb], w=[lgb])
                        POP("scalar", lambda h: h.activation(out=lg[:, 16:24], in_=lg[:, 0:8], func=AF.Exp, bias=lg[:, 24:25]),
                             r=[lgb], w=[lgb])
                        POP("vector", lambda h: h.scalar_tensor_tensor(lg[:, 16:24], lg[:, 0:8], lg[:, 9:10], lg[:, 16:24],
                                                                       ALU.is_ge, ALU.mult), r=[lgb], w=[lgb])
                        POP("vector", lambda h: h.tensor_reduce(lg[:, 25:26], lg[:, 16:24], AX.X, ALU.add), r=[lgb], w=[lgb])
                        POP("vector", lambda h: h.reciprocal(lg[:, 26:27], lg[:, 25:26]), r=[lgb], w=[lgb])
                        POP("vector", lambda h: h.tensor_scalar(gates[:, tt, :], lg[:, 16:24], lg[:, 26:27], None, ALU.mult),
                             r=[lgb], w=[B_gates[tt]])
                        if "lg_d" in dump:
                            P.dma("sync", lg_dd[:, t, :], lg[:], r=[lgb], w=[Buf()])
                            if t == 0:
                                P.dma("sync", h32_dd[:, :, :], h32[:], r=[h32b], w=[Buf()])
                if n_exp > 1 and "lg_d" in dump and 'router' not in skip:
                    pass
                if n_exp > 1 and "gates_d" in dump:
                    P.dma("sync", gates_dd[:, g * 8:(g + 1) * 8, :], gates[:], r=B_gates, w=[Buf()])
                for e in range(n_exp if n_exp == 1 else moe_nexp):
                    wgv = wg[e].rearrange("(kc p) f -> p kc f", p=128)
                    wuv = wu[e].rearrange("(kc p) f -> p kc f", p=128)
                    for si, sg_ in enumerate(subgroups):
                        nch = sum(w_ // 128 for _, w_ in sg_)
                        wdt_, wdb = wdr.next()
                        ch = 0
                        for (f0, fw) in sg_:
                            P.dma("gpsimd", wdt_[:, ch:ch + fw // 128, :],
                                  wd[e, f0:f0 + fw, :].rearrange("(fc p) d -> p fc d", p=128), w=[wdb])
                            ch += fw // 128
                        ch = 0
                        for (f0, fw) in sg_:
                            wgt, wgb = wgr.next()
                            wut, wub = wur.next()
                            P.dma("gpsimd", wgt[:, :, 0:fw], wgv[:, :, f0:f0 + fw], w=[wgb])
                            P.dma("gpsimd", wut[:, :, 0:fw], wuv[:, :, f0:f0 + fw], w=[wub])
                            for fc in range(fw // 128):
                                for nt in range(G // 512):
                                    pg, pgb = psg.next()
                                    pu, pub = psu.next()

                                    def fn(h, pg=pg, pu=pu, wgt=wgt, wut=wut, fc=fc, nt=nt):
                                        for kc in range(8):
                                            h.matmul(pg[:], wgt[:, kc, fc * 128:(fc + 1) * 128], hT[:, kc, nt * 512:(nt + 1) * 512],
                                                     start=(kc == 0), stop=(kc == 7))
                                        for kc in range(8):
                                            ins = h.matmul(pu[:], wut[:, kc, fc * 128:(fc + 1) * 128], hT[:, kc, nt * 512:(nt + 1) * 512],
                                                           start=(kc == 0), stop=(kc == 7))
                                        return ins
                                    P.op("tensor", fn, r=[wgb, wub] + B_hT[nt * 4:(nt + 1) * 4], w=[pgb, pub])
                                    sgt, sgb = sgr.next()
                                    P.op("scalar", lambda h, pg=pg, sgt=sgt: h.activation(out=sgt[:], in_=pg[:], func=AF.Silu),
                                         r=[pgb], w=[sgb])
                                    P.op("vector", lambda h, pu=pu, sgt=sgt, ch=ch, fc=fc, nt=nt: h.tensor_tensor(
                                        actT[:, ch + fc, nt * 512:(nt + 1) * 512], sgt[:], pu[:], ALU.mult),
                                        r=[sgb, pub], w=[B_act])
                            ch += fw // 128
                        for tt in range(8):
                            for half in range(2):
                                pd, pdb = psd.next()

                                def fn(h, pd=pd, tt=tt, half=half, wdt_=wdt_, nch=nch):
                                    for cc in range(nch):
                                        ins = h.matmul(pd[:], actT[:, cc, tt * 128:(tt + 1) * 128], wdt_[:, cc, half * 512:(half + 1) * 512],
                                                       start=(cc == 0), stop=(cc == nch - 1))
                                    return ins
                                P.op("tensor", fn, r=[B_act, wdb], w=[pdb])
                                ysl = ysum[:, tt, half * 512:(half + 1) * 512]
                                first = (e == 0 and si == 0)
                                if n_exp > 1:
                                    gap = gates[:, tt, e:e + 1]
                                    if first:
                                        P.op("vector", lambda h, pd=pd, ysl=ysl, gap=gap: h.tensor_scalar(ysl, pd[:], gap, None, ALU.mult),
                                             r=[pdb, B_gates[tt]], w=[B_ys[tt]])
                                    else:
                                        P.op("vector", lambda h, pd=pd, ysl=ysl, gap=gap: h.scalar_tensor_tensor(
                                            ysl, pd[:], gap, ysl, ALU.mult, ALU.add), r=[pdb, B_gates[tt], B_ys[tt]], w=[B_ys[tt]])
                                else:
                                    if first:
                                        P.op("vector", lambda h, pd=pd, ysl=ysl: h.tensor_copy(ysl, pd[:]), r=[pdb], w=[B_ys[tt]])
                                    else:
                                        P.op("vector", lambda h, pd=pd, ysl=ysl: h.tensor_tensor(ysl, ysl, pd[:], ALU.add),
                                             r=[pdb, B_ys[tt]], w=[B_ys[tt]])
                for tt in range(8):
                    t = g * 8 + tt
                    xt, xb = ncx.xr.next()
                    P.dma("sync", xt[:], xsrc[t * 128:(t + 1) * 128, :], r=[xbufs[t]] if xbufs else [], w=[xb])
                    P.op("vector", lambda h: h.tensor_tensor(ysum[:, tt, :], ysum[:, tt, :], gbc[:], ALU.mult),
                         r=[B_ys[tt], b_g], w=[B_ys[tt]])
                    P.op("vector", lambda h: h.tensor_tensor(xt[:], xt[:], ysum[:, tt, :], ALU.add), r=[B_ys[tt], xb], w=[xb])
                    if final and 'final' not in skip:
                        jt, jb = ncx.jr.next()
                        st, stb = ncx.sr.next()
                        P.op("vector", lambda h: h.memset(st[:], 0.0), w=[stb])
                        P.op("scalar", lambda h: h.activation(out=jt[:], in_=xt[:], func=AF.Square, accum_out=st[:, 0:1]),
                             r=[xb], w=[jb, stb])
                        P.op("scalar", lambda h: h.activation(out=st[:, 1:2], in_=st[:, 0:1], func=AF.Sqrt, scale=1.0 / D,
                                                              bias=EPS_AP[:, 0:1]), r=[stb, B_eps], w=[stb])
                        P.op("vector", lambda h: h.reciprocal(st[:, 2:3], st[:, 1:2]), r=[stb], w=[stb])
                        P.op("vector", lambda h: h.scalar_tensor_tensor(xt[:], xt[:], st[:, 2:3], lnf[:], ALU.mult, ALU.mult),
                             r=[xb, stb, b_lnf], w=[xb])
                    P.dma("sync", xdst[t * 128:(t + 1) * 128, :], xt[:], r=[xb], w=[xdbufs[t]])
        P.barrier()

    def lru_phase():
        with ExitStack() as c:
            lv = sb(c, "lv", [128, 4, 8])
            B_lv = Buf()
            P.dma("sync", lv[:], lru_vec[:, :, :], w=[B_lv])
            c8 = sb(c, "c8", [128, 4, 4])
            B_c8 = Buf()
            P.op("scalar", lambda h: h.activation(out=c8[:, :, 2], in_=lv[:, :, 7], func=AF.Exp, scale=-1.0), r=[B_lv], w=[B_c8])
            P.op("scalar", lambda h: h.activation(out=c8[:, :, 3], in_=c8[:, :, 2], func=AF.Ln, bias=1.0), r=[B_c8], w=[B_c8])
            P.op("vector", lambda h: h.tensor_scalar(c8[:, :, 0], c8[:, :, 3], -8.0, None, ALU.mult), r=[B_c8], w=[B_c8])
            P.op("vector", lambda h: h.tensor_scalar(c8[:, :, 1], c8[:, :, 3], -16.0, None, ALU.mult), r=[B_c8], w=[B_c8])
            gA = sb(c, "gA", [128, 4, 128], BF16)
            gX = sb(c, "gX", [128, 4, 128], BF16)
            B_gw = Buf()
            P.op("vector", lambda h: h.memset(gA[:], 0.0), w=[B_gw])
            P.op("vector", lambda h: h.memset(gX[:], 0.0), w=[B_gw])
            for cc in range(4):
                for j in range(2):
                    P.dma("gpsimd", gA[j * 64:(j + 1) * 64, cc, j * 64:(j + 1) * 64], lru_ga[2 * cc + j, :, :], w=[B_gw])
                    P.dma("gpsimd", gX[j * 64:(j + 1) * 64, cc, j * 64:(j + 1) * 64], lru_gx[2 * cc + j, :, :], w=[B_gw])
            XBr = Ring([sb(c, "lxb%d" % i, [128, S + 4]) for i in range(2)])
            GBr = Ring([sb(c, "lgb%d" % i, [128, S]) for i in range(2)])
            YBr = Ring([sb(c, "lyb%d" % i, [128, S], BF16) for i in range(2)])
            mk = lambda nm, n, dt=F32: Ring([sb(c, "%s%d" % (nm, i), [128, 512], dt) for i in range(n)])
            XCr, XCbr, Rr, Ir, A2r, Ur, Hr, GGr = mk("lxc", 2), mk("lxcb", 2, BF16), mk("lr", 2), mk("li", 2), mk("la2", 2), \
                mk("lu", 2), mk("lh", 3), mk("lgg", 2)
            psr = Ring([ps(c, "lpr%d" % i, [128, 512]) for i in range(2)])
            psi = Ring([ps(c, "lpi%d" % i, [128, 512]) for i in range(2)])
            for cc in range(4):
                xbt, xbb = XBr.next()
                gbt, gbb = GBr.next()
                ybt, ybb = YBr.next()
                P.op("vector", lambda h: h.memset(xbt[:, 0:4], 0.0), w=[xbb])
                P.dma("sync", xbt[:, 4:S + 4], xg_d[cc, :, :], r=[B_xg[cc]], w=[xbb])
                P.dma("scalar", gbt[:], xg_d[4 + cc, :, :], r=[B_xg[4 + cc]], w=[gbb])
                hprev = None
                for nt in range(8):
                    o = nt * 512
                    xc, xcb = XCr.next()
                    P.op("vector", lambda h: h.tensor_scalar(xc[:], xbt[:, o + 1:o + 513], lv[:, cc, 0:1], lv[:, cc, 4:5], ALU.mult, ALU.add),
                         r=[xbb, B_lv], w=[xcb])
                    for i in range(1, 4):
                        P.op("vector", lambda h, i=i: h.scalar_tensor_tensor(xc[:], xbt[:, o + 1 + i:o + 513 + i], lv[:, cc, i:i + 1], xc[:],
                                                                             ALU.mult, ALU.add), r=[xbb, B_lv, xcb], w=[xcb])
                    xcbf, xcbfb = XCbr.next()
                    P.op("gpsimd", lambda h: h.tensor_copy(xcbf[:], xc[:]), r=[xcb], w=[xcbfb])
                    pr_, prb = psr.next()
                    pi_, pib = psi.next()

                    def fn(h):
                        h.matmul(pr_[:], gA[:, cc, :], xcbf[:], start=True, stop=True)
                        return h.matmul(pi_[:], gX[:, cc, :], xcbf[:], start=True, stop=True)
                    P.op("tensor", fn, r=[B_gw, xcbfb], w=[prb, pib])
                    rt, rb_ = Rr.next()
                    it, ib_ = Ir.next()
                    P.op("scalar", lambda h: h.activation(out=rt[:], in_=pr_[:], func=AF.Sigmoid, bias=lv[:, cc, 5:6]), r=[prb, B_lv], w=[rb_])
                    P.op("scalar", lambda h: h.activation(out=it[:], in_=pi_[:], func=AF.Sigmoid, bias=lv[:, cc, 6:7]), r=[pib, B_lv], w=[ib_])
                    a2, a2b = A2r.next()
                    P.op("scalar", lambda h: h.activation(out=a2[:], in_=rt[:], func=AF.Exp, scale=c8[:, cc, 1:2]), r=[rb_, B_c8], w=[a2b])
                    P.op("scalar", lambda h: h.activation(out=rt[:], in_=rt[:], func=AF.Exp, scale=c8[:, cc, 0:1]), r=[rb_, B_c8], w=[rb_])
                    P.op("scalar", lambda h: h.activation(out=a2[:], in_=a2[:], func=AF.Sqrt, scale=-1.0, bias=1.0), r=[a2b], w=[a2b])
                    gg, ggb = GGr.next()
                    P.op("scalar", lambda h: h.activation(out=gg[:], in_=gbt[:, o:o + 512], func=AF.Gelu), r=[gbb], w=[ggb])
                    ut, ub_ = Ur.next()
                    P.op("vector", lambda h: h.tensor_tensor(ut[:], a2[:], it[:], ALU.mult), r=[a2b, ib_], w=[ub_])
                    P.op("vector", lambda h: h.tensor_tensor(ut[:], ut[:], xc[:], ALU.mult), r=[ub_, xcb], w=[ub_])
                    ht_, hb_ = Hr.next()
                    if hprev is None:
                        P.op("vector", lambda h: h.tensor_tensor_scan(ht_[:], rt[:], ut[:], 0.0, ALU.mult, ALU.add), r=[rb_, ub_], w=[hb_])
                    else:
                        hp, hpb = hprev
                        P.op("vector", lambda h: h.tensor_tensor_scan(ht_[:], rt[:], ut[:], hp[:, 511:512], ALU.mult, ALU.add),
                             r=[rb_, ub_, hpb], w=[hb_])
                    hprev = (ht_, hb_)
                    P.op("vector", lambda h: h.tensor_tensor(ybt[:, o:o + 512], ht_[:], gg[:], ALU.mult), r=[hb_, ggb], w=[ybb])
                P.dma("sync", yT_d[4 + cc, :, :], ybt[:], r=[ybb], w=[B_yT[4 + cc]])
        P.barrier()

    def sb_attn_phase():
        with ExitStack() as c:
            lmask = sb(c, "lmask_sb", [128, 128])
            B_lm = Buf()
            P.dma("sync", lmask[:], lmask_d[:, :], w=[B_lm])
            lmask16 = sb(c, "lmask16", [128, 128], BF16)
            P.op("vector", lambda h: h.tensor_copy(lmask16[:], lmask[:]), r=[B_lm], w=[B_lm])
            onesw = sb(c, "onesw", [128, 512])
            B_on = Buf()
            P.op("vector", lambda h: h.memset(onesw[:], 1.0), w=[B_on])
            yaT = sb(c, "yaT", [128, 4, S], BF16)
            B_yaT = Buf()
            QTr = Ring([sb(c, "sq%d" % i, [64, S], BF16) for i in range(2)])
            KTr = Ring([sb(c, "sk%d" % i, [64, S], BF16) for i in range(2)])
            Vr = Ring([sb(c, "sv%d" % i, [128, NT, 64], BF16) for i in range(2)])
            mk = lambda nm, n, dt=F32, w_=512: Ring([sb(c, "%s%d" % (nm, i), [128, w_], dt) for i in range(n)])
            Er, SPr, PFr, T2r, Wr, WTr, NCr = mk("sE", 3), mk("sSP", 5), mk("sPF", 5), mk("sT2", 4), mk("sW", 4, BF16), \
                mk("sWT", 4, BF16), mk("sNC", 14, F32, 2)
            YA = sb(c, "sYA", [128, NT, 512])
            B_YA = bufs(NT)
            tiles = []
            for hd in range(8):
                for qb in range(NT):
                    L = (qb + 1) * 128
                    nkt = (L + 511) // 512
                    for kt in range(nkt - 1, -1, -1):
                        tiles.append(dict(hd=hd, qb=qb, kt=kt, W=min(512, L - kt * 512), diag=(kt == nkt - 1),
                                          first=(kt == nkt - 1), last=(kt == 0), nblk=L // 128))
            heads = {}

            def load_head(hd):
                if hd in heads or hd > 7:
                    return
                qt, qb_ = QTr.next()
                kt_, kb_ = KTr.next()
                vt, vb_ = Vr.next()
                po = (hd % 2) * 64
                P.dma("sync", qt[:], qk_d[hd // 2, po:po + 64, :], r=[B_qk[hd // 2]], w=[qb_])
                P.dma("scalar", kt_[:], qk_d[4 + hd // 2, po:po + 64, :], r=[B_qk[4 + hd // 2]], w=[kb_])
                P.dma("sync", vt[:], v_d[:, hd * 64:(hd + 1) * 64].rearrange("(b p) d -> p b d", p=128), r=B_v, w=[vb_])
                heads[hd] = (qt, qb_, kt_, kb_, vt, vb_)

            with ExitStack() as c2:
                psZ = Ring([ps(c2, "spz%d" % i, [128, 512]) for i in range(4)])
                psT = Ring([ps(c2, "spt%d" % i, [128, 1024], BF16)[:, 0:512] for i in range(3)])
                psO = Ring([ps(c2, "spo%d" % i, [128, 512])[:, 0:64] for i in range(1)])
                st = {"nc": None, "po": None, "blk": 0}

                def stA(T):
                    hd, qb, kt, W = T["hd"], T["qb"], T["kt"], T["W"]
                    if hd not in heads:
                        load_head(hd)
                    if T["first"] and qb == 0:
                        load_head(hd + 1)
                    qt, qb_, kt_, kb_, vt, vb_ = heads[hd]
                    pz, pzb = psZ.next()
                    P.op("tensor", lambda h: h.matmul(pz[:, 0:W], qt[:, qb * 128:(qb + 1) * 128], kt_[:, kt * 512:kt * 512 + W],
                                                      start=True, stop=True), r=[qb_, kb_], w=[pzb])
                    et, eb = Er.next()
                    spt, spb = SPr.next()
                    P.op("scalar", lambda h: h.activation(out=et[:, 0:W], in_=pz[:, 0:W], func=AF.Exp, scale=0.125), r=[pzb], w=[eb])
                    P.op("scalar", lambda h: h.activation(out=spt[:, 0:W], in_=et[:, 0:W], func=AF.Ln, bias=1.0), r=[eb], w=[spb])
                    T["pz"], T["pzb"], T["sp"], T["spb"] = pz, pzb, spt, spb

                def stB(T):
                    W = T["W"]
                    pz, pzb, spt, spb = T["pz"], T["pzb"], T["sp"], T["spb"]
                    if T["diag"]:
                        P.op("vector", lambda h: h.tensor_tensor(spt[:, W - 128:W], spt[:, W - 128:W], lmask[:], ALU.mult),
                             r=[spb, B_lm], w=[spb])
                    pf, pfb = PFr.next()
                    P.op("vector", lambda h: h.tensor_tensor_scan(pf[:, 0:W], onesw[:, 0:W], spt[:, 0:W], 0.0, ALU.mult, ALU.add),
                         r=[spb, B_on], w=[pfb])
                    nct2, ncb2 = NCr.next()
                    if T["first"]:
                        P.op("vector", lambda h: h.tensor_scalar(nct2[:, 0:1], pf[:, W - 1:W], -1.0, None, ALU.mult), r=[pfb], w=[ncb2])
                    else:
                        nct, ncb = st["nc"]
                        P.op("vector", lambda h: h.tensor_tensor(nct2[:, 0:1], nct[:, 0:1], pf[:, W - 1:W], ALU.subtract),
                             r=[ncb, pfb], w=[ncb2])
                    st["nc"] = (nct2, ncb2)
                    P.op("gpsimd", lambda h: h.tensor_tensor(pf[:, 0:W], pf[:, 0:W], spt[:, 0:W], ALU.subtract), r=[pfb, spb], w=[pfb])
                    T["pf"], T["pfb"], T["nc"] = pf, pfb, (nct2, ncb2)

                def stB2(T):
                    W = T["W"]
                    pz, pzb, pf, pfb = T["pz"], T["pzb"], T["pf"], T["pfb"]
                    nct2, ncb2 = T["nc"]
                    t2, t2b = T2r.next()
                    P.op("vector", lambda h: h.scalar_tensor_tensor(t2[:, 0:W], pz[:, 0:W], 0.125, pf[:, 0:W], ALU.mult, ALU.add),
                         r=[pzb, pfb], w=[t2b])
                    T["t2"], T["t2b"], T["nc"] = t2, t2b, (nct2, ncb2)

                def stC(T):
                    hd, qb, kt, W = T["hd"], T["qb"], T["kt"], T["W"]
                    qt, qb_, kt_, kb_, vt, vb_ = heads[hd]
                    t2, t2b = T["t2"], T["t2b"]
                    nct, ncb = T["nc"]
                    wt_, wb_ = Wr.next()
                    P.op("scalar", lambda h: h.activation(out=wt_[:, 0:W], in_=t2[:, 0:W], func=AF.Exp, bias=nct[:, 0:1]),
                         r=[t2b, ncb], w=[wb_])
                    if T["diag"]:
                        P.op("gpsimd", lambda h: h.tensor_tensor(wt_[:, W - 128:W], wt_[:, W - 128:W], lmask16[:], ALU.mult),
                             r=[wb_, B_lm], w=[wb_])
                    nb = W // 128
                    pt_, ptb = psT.next()

                    def fnT(h):
                        for j in range(nb):
                            ins = h.transpose(pt_[:, j * 128:(j + 1) * 128], wt_[:, j * 128:(j + 1) * 128], ident16[:])
                        return ins
                    P.op("tensor", fnT, r=[wb_, B_id16], w=[ptb])
                    T["pt"], T["ptb"], T["nb"] = pt_, ptb, nb

                def stD(T):
                    hd, qb, kt, W = T["hd"], T["qb"], T["kt"], T["W"]
                    qt, qb_, kt_, kb_, vt, vb_ = heads[hd]
                    pt_, ptb, nb = T["pt"], T["ptb"], T["nb"]
                    wT, wTb = WTr.next()
                    P.op("vector", lambda h: h.tensor_copy(wT[:, 0:W], pt_[:, 0:W]), r=[ptb], w=[wTb])
                    T["wT"], T["wTb"] = wT, wTb

                def stE(T):
                    hd, qb, kt, W = T["hd"], T["qb"], T["kt"], T["W"]
                    qt, qb_, kt_, kb_, vt, vb_ = heads[hd]
                    wT, wTb, nb = T["wT"], T["wTb"], T["nb"]
                    if T["first"]:
                        st["po"] = psO.next()
                        st["blk"] = 0
                    po_, pob = st["po"]
                    nblk_total = T["nblk"]

                    def fnO(h):
                        for j in range(nb):
                            ins = h.matmul(po_[:], wT[:, j * 128:(j + 1) * 128], vt[:, kt * 4 + j, :],
                                           start=(st["blk"] == 0), stop=(st["blk"] == nblk_total - 1))
                            st["blk"] += 1
                        return ins
                    P.op("tensor", fnO, r=[wTb, vb_], w=[pob])
                    if T["last"]:
                        P.op("scalar", lambda h: h.copy(YA[:, qb, hd * 64:(hd + 1) * 64], po_[:]), r=[pob], w=[B_YA[qb]])

                n = len(tiles)
                for s_ in range(n + 5):
                    if 0 <= s_ - 5 < n:
                        stE(tiles[s_ - 5])
                    if 0 <= s_ - 4 < n:
                        stD(tiles[s_ - 4])
                    if s_ < n:
                        stA(tiles[s_])
                    if 0 <= s_ - 1 < n:
                        stB(tiles[s_ - 1])
                    if 0 <= s_ - 2 < n:
                        stB2(tiles[s_ - 2])
                    if 0 <= s_ - 3 < n:
                        stC(tiles[s_ - 3])
            P.barrier()
            psY = Ring([ps(c, "spy%d" % i, [128, 512])[:, 0:128] for i in range(2)])
            k_ = 0
            for qb in range(NT):
                for ch in range(4):
                    py, pyb = psY.next()
                    P.op("tensor", lambda h: h.transpose(py[:], YA[:, qb, ch * 128:(ch + 1) * 128], ident[:]), r=[B_YA[qb], B_ident], w=[pyb])
                    k_ += 1
                    if k_ % 2:
                        P.op("scalar", lambda h: h.copy(yaT[:, ch, qb * 128:(qb + 1) * 128], py[:]), r=[pyb], w=[B_yaT])
                    else:
                        P.op("vector", lambda h: h.tensor_copy(yaT[:, ch, qb * 128:(qb + 1) * 128], py[:]), r=[pyb], w=[B_yaT])
            for ch in range(4):
                P.dma("sync", yT_d[ch, :, :], yaT[:, ch, :], r=[B_yaT], w=[B_yT[ch]])
        P.barrier()

    ident16 = sb(ctx, "ident16", [128, 128], BF16)
    B_id16 = Buf()
    P.op("vector", lambda h: h.tensor_copy(ident16[:], ident[:]), r=[B_ident], w=[B_id16])

    def diff_attn_phase():
        with ExitStack() as c:
            rbt = sb(c, "rbt", [128, 256])
            B_rb = Buf()
            P.dma("sync", rbt[:], rel_bias[0:1, :].to_broadcast([128, 256]), w=[B_rb])
            bk = sb(c, "bk_sb", [128, 256])
            B_bk = Buf()
            P.dma("sync", bk[:], bk_d[:, :], w=[B_bk])
            negm = sb(c, "negm_sb", [128, 128])
            B_ng = Buf()
            P.dma("sync", negm[:], negm_d[:, :], w=[B_ng])
            TB = sb(c, "TB", [128, 8, 256])
            B_TB = Buf()
            tmpr = Ring([sb(c, "tmpb%d" % i, [128, 256]) for i in range(2)])
            for hd in range(8):
                for b in range(32):
                    idx = b * 8 + hd
                    if b == 0:
                        P.op("vector", lambda h: h.tensor_scalar(TB[:, hd, :], bk[:], float(b), rbt[:, idx:idx + 1], ALU.is_equal, ALU.mult),
                             r=[B_bk, B_rb], w=[B_TB])
                    else:
                        tmpb, B_tmp = tmpr.next()
                        P.op("vector", lambda h: h.tensor_scalar(tmpb[:], bk[:], float(b), rbt[:, idx:idx + 1], ALU.is_equal, ALU.mult),
                             r=[B_bk, B_rb], w=[B_tmp])
                        P.op("vector", lambda h: h.tensor_tensor(TB[:, hd, :], TB[:, hd, :], tmpb[:], ALU.add), r=[B_tmp, B_TB], w=[B_TB])
                P.op("vector", lambda h: h.tensor_tensor(TB[:, hd, 128:256], TB[:, hd, 128:256], negm[:], ALU.add), r=[B_ng, B_TB], w=[B_TB])
            dlt = sb(c, "dlt", [128, 256])
            B_dl = Buf()
            P.dma("sync", dlt[:], dl[0:1, :].to_broadcast([128, 256]), w=[B_dl])
            lamt = sb(c, "lamt", [128, 8])
            P.op("vector", lambda h: h.tensor_tensor(dlt[:, 0:64], dlt[:, 0:64], dlt[:, 64:128], ALU.mult), r=[B_dl], w=[B_dl])
            P.op("vector", lambda h: h.tensor_tensor(dlt[:, 128:192], dlt[:, 128:192], dlt[:, 192:256], ALU.mult), r=[B_dl], w=[B_dl])
            P.op("vector", lambda h: h.tensor_reduce(lamt[:, 0:1], dlt[:, 0:64], AX.X, ALU.add), r=[B_dl], w=[B_dl])
            P.op("vector", lambda h: h.tensor_reduce(lamt[:, 1:2], dlt[:, 128:192], AX.X, ALU.add), r=[B_dl], w=[B_dl])
            P.op("scalar", lambda h: h.activation(out=lamt[:, 2:4], in_=lamt[:, 0:2], func=AF.Exp), r=[B_dl], w=[B_dl])
            P.op("vector", lambda h: h.tensor_tensor(lamt[:, 4:5], lamt[:, 3:4], lamt[:, 2:3], ALU.subtract), r=[B_dl], w=[B_dl])
            P.op("vector", lambda h: h.tensor_scalar(lamt[:, 5:6], lamt[:, 4:5], -LAMBDA_INIT, None, ALU.add), r=[B_dl], w=[B_dl])
            sgt = sb(c, "sgt", [128, 128])
            B_sg = Buf()
            P.dma("sync", sgt[:], subln[0:1, :].to_broadcast([128, 128]), w=[B_sg])
            P.op("vector", lambda h: h.tensor_scalar(sgt[:], sgt[:], 1.0 - LAMBDA_INIT, None, ALU.mult), r=[B_sg], w=[B_sg])

            ones64 = sb(c, "ones64", [64, 1], BF16)
            B_o64 = Buf()
            P.op("vector", lambda h: h.memset(ones64[:], 1.0), w=[B_o64])
            QT = [sb(c, "dq%d" % m, [64, S], BF16) for m in range(2)]
            KT = [sb(c, "dk%d" % m, [64, S], BF16) for m in range(2)]
            B_Q = bufs(2)
            B_K = bufs(2)
            sq = sb(c, "dsq", [64, S], BF16)
            B_sq = Buf()
            Vr = Ring([sb(c, "dv%d" % i, [128, NT, 128], BF16) for i in range(2)])
            Pr = Ring([sb(c, "dP%d" % i, [128, S], BF16) for i in range(4)])
            Ar = Ring([sb(c, "dA%d" % i, [128, S], BF16) for i in range(3)])
            ATr = Ring([sb(c, "dAT%d" % i, [128, NT, 128], BF16) for i in range(3)])
            str_ = Ring([sb(c, "dst%d" % i, [128, 40]) for i in range(8)])
            tsr = Ring([sb(c, "dts%d" % i, [128, 128]) for i in range(8)])
            Or = Ring([sb(c, "dO%d" % i, [128, 128]) for i in range(2)])
            jr = Ring([sb(c, "dj%d" % i, [128, 128]) for i in range(1)])
            oT = Ring([sb(c, "doT%d" % i, [128, S], BF16) for i in range(2)])
            NSH = [sb(c, "dnsh%d" % m, [128, 32]) for m in range(2)]
            NSF = [sb(c, "dnsf%d" % m, [128, 32]) for m in range(2)]
            B_NS = bufs(2)
            km = sb(c, "dkm", [128, 16])
            B_km = Buf()
            mb = sb(c, "dmb", [128, 2])
            B_mb = Buf()
            psZ = Ring([ps(c, "dpz%d" % i, [128, 512]) for i in range(3)])
            psT = Ring([ps(c, "dpt%d" % i, [128, 1024], BF16)[:, 0:512] for i in range(2)])
            psO = Ring([ps(c, "dpo%d" % i, [128, 512])[:, 0:128] for i in range(1)])
            psY = Ring([ps(c, "dpy%d" % i, [128, 512])[:, 0:128] for i in range(1)])
            psS = ps(c, "dpsS", [128, 512])
            B_psS = Buf()
            hstate = {}

            def load_head(hd):
                cfar = rbt[:, 31 * 8 + hd:31 * 8 + hd + 1]
                P.op("vector", lambda h: h.tensor_reduce(mb[:, 0:1], rbt[:, hd:256:8], AX.X, ALU.max), r=[B_rb], w=[B_mb])
                P.op("vector", lambda h: h.tensor_scalar(mb[:, 1:2], mb[:, 0:1], -1.0, None, ALU.mult), r=[B_mb], w=[B_mb])
                for m in range(2):
                    P.dma("sync", QT[m][:], qk_d[hd, m * 64:(m + 1) * 64, :], r=[B_qk[hd]], w=[B_Q[m]])
                    P.dma("scalar", KT[m][:], qk_d[8 + hd, m * 64:(m + 1) * 64, :], r=[B_qk[8 + hd]], w=[B_K[m]])
                    P.op("scalar", lambda h: h.activation(out=sq[:], in_=KT[m][:], func=AF.Square), r=[B_K[m]], w=[B_sq])
                    for t8 in range(8):
                        P.op("tensor", lambda h: h.matmul(psS[0:1, 0:512], ones64[:, 0:1], sq[:, t8 * 512:(t8 + 1) * 512], start=True, stop=True),
                             r=[B_sq, B_o64], w=[B_psS])
                        P.op("vector", lambda h: h.tensor_reduce(km[0:1, t8:t8 + 1], psS[0:1, 0:512], AX.X, ALU.max), r=[B_psS], w=[B_km])
                    P.op("vector", lambda h: h.tensor_reduce(km[0:1, 8:9], km[0:1, 0:8], AX.X, ALU.max), r=[B_km], w=[B_km])
                    P.op("tensor", lambda h: h.matmul(psS[:, 0:1], ones1[0:1, :], km[0:1, 8:9], start=True, stop=True), r=[B_km, B_ones1], w=[B_psS])
                    P.op("vector", lambda h: h.tensor_copy(km[:, 10:11], psS[:, 0:1]), r=[B_psS], w=[B_km])
                    P.op("scalar", lambda h: h.activation(out=sq[:], in_=QT[m][:], func=AF.Square), r=[B_Q[m], B_sq], w=[B_sq])

                    def fnq(h):
                        for qb in range(NT):
                            ins = h.matmul(psS[:, 32 + qb:33 + qb], sq[:, qb * 128:(qb + 1) * 128], ones64[:, 0:1], start=True, stop=True)
                        return ins
                    P.op("tensor", fnq, r=[B_sq, B_o64], w=[B_psS])
                    P.op("vector", lambda h: h.tensor_scalar(NSH[m][:], psS[:, 32:64], km[:, 10:11], None, ALU.mult), r=[B_psS, B_km], w=[B_NS[m]])
                    P.op("scalar", lambda h: h.activation(out=NSH[m][:], in_=NSH[m][:], func=AF.Sqrt), r=[B_NS[m]], w=[B_NS[m]])
                    P.op("vector", lambda h: h.tensor_scalar(NSH[m][:], NSH[m][:], -0.13, None, ALU.mult), r=[B_NS[m]], w=[B_NS[m]])
                    P.op("vector", lambda h: h.tensor_scalar(NSH[m][:], NSH[m][:], mb[:, 1:2], None, ALU.add), r=[B_NS[m], B_mb], w=[B_NS[m]])
                    P.op("vector", lambda h: h.tensor_scalar(NSF[m][:], NSH[m][:], cfar, None, ALU.add), r=[B_NS[m], B_rb], w=[B_NS[m]])
                vt, vb_ = Vr.next()
                P.dma("sync", vt[:], v_d[:, hd * 128:(hd + 1) * 128].rearrange("(b p) d -> p b d", p=128), r=B_v, w=[vb_])
                ot, otb = oT.next()
                hstate[hd] = (vt, vb_, ot, otb)

            units = [dict(hd=hd, qb=qb) for hd in range(8) for qb in range(NT)]
            cnt = {"k": 0}

            def stA(U):
                hd, qb = U["hd"], U["qb"]
                if qb == 0:
                    load_head(hd)
                L = (qb + 1) * 128
                nkt = (L + 511) // 512
                st, stb = str_.next()
                P.op("vector", lambda h: h.memset(st[:], 0.0), w=[stb])
                Pm = []
                for m in range(2):
                    Pt, Ptb = Pr.next()
                    for kt in range(nkt):
                        W = min(512, L - kt * 512)
                        pz, pzb = psZ.next()
                        P.op("tensor", lambda h: h.matmul(pz[:, 0:W], QT[m][:, qb * 128:(qb + 1) * 128], KT[m][:, kt * 512:kt * 512 + W],
                                                          start=True, stop=True), r=[B_Q[m], B_K[m]], w=[pzb])
                        c_lo = kt * 512
                        c_hi = c_lo + W
                        far_hi = min(c_hi, max(c_lo, L - 256))
                        near = []
                        for (b_lo, tb_lo, col) in ((L - 256, 0, 8), (L - 128, 128, 9)):
                            if b_lo >= c_lo and b_lo < c_hi and b_lo >= 0:
                                o_ = b_lo - c_lo
                                ts, tsb = tsr.next()
                                P.op("vector", lambda h: h.scalar_tensor_tensor(ts[:], pz[:, o_:o_ + 128], 0.125, TB[:, hd, tb_lo:tb_lo + 128],
                                                                               ALU.mult, ALU.add), r=[pzb, B_TB], w=[tsb])
                                near.append((b_lo, col, ts, tsb))
                        if far_hi > c_lo:
                            n_ = far_hi - c_lo
                            P.op("scalar", lambda h: h.activation(out=Pt[:, c_lo:far_hi], in_=pz[:, 0:n_], func=AF.Exp, scale=0.125,
                                                                  bias=NSF[m][:, qb:qb + 1], accum_out=st[:, m * 10 + kt:m * 10 + kt + 1]),
                                 r=[pzb, B_NS[m]] + [x[3] for x in near], w=[Ptb, stb])
                        for (b_lo, col, ts, tsb) in near:
                            P.op("scalar", lambda h: h.activation(out=Pt[:, b_lo:b_lo + 128], in_=ts[:], func=AF.Exp,
                                                                  bias=NSH[m][:, qb:qb + 1], accum_out=st[:, m * 10 + col:m * 10 + col + 1]),
                                 r=[tsb, B_NS[m]], w=[Ptb, stb])
                    Pm.append((Pt, Ptb))
                U["st"], U["stb"], U["Pm"], U["L"] = st, stb, Pm, L

            def stB(U):
                st, stb, Pm, L = U["st"], U["stb"], U["Pm"], U["L"]
                P.op("vector", lambda h: h.tensor_reduce(st[:, 20:21], st[:, 0:10], AX.X, ALU.add), r=[stb], w=[stb])
                P.op("vector", lambda h: h.tensor_reduce(st[:, 21:22], st[:, 10:20], AX.X, ALU.add), r=[stb], w=[stb])
                P.op("vector", lambda h: h.reciprocal(st[:, 22:24], st[:, 20:22]), r=[stb], w=[stb])
                P.op("vector", lambda h: h.tensor_tensor(st[:, 24:25], st[:, 23:24], lamt[:, 5:6], ALU.mult), r=[stb, B_dl], w=[stb])
                At, Atb = Ar.next()
                P.op("vector", lambda h: h.tensor_scalar(At[:, 0:L], Pm[0][0][:, 0:L], st[:, 22:23], None, ALU.mult),
                     r=[Pm[0][1], stb], w=[Atb])
                P.op("vector", lambda h: h.scalar_tensor_tensor(At[:, 0:L], Pm[1][0][:, 0:L], st[:, 24:25], At[:, 0:L], ALU.mult, ALU.add),
                     r=[Pm[1][1], stb, Atb], w=[Atb])
                U["At"], U["Atb"] = At, Atb

            def stC(U):
                At, Atb, L = U["At"], U["Atb"], U["L"]
                ATt, ATb = ATr.next()
                nblk = L // 128
                for g4 in range((nblk + 3) // 4):
                    nb = min(4, nblk - g4 * 4)
                    pt_, ptb = psT.next()

                    def fnT(h):
                        for j in range(nb):
                            cb = (g4 * 4 + j) * 128
                            ins = h.transpose(pt_[:, j * 128:(j + 1) * 128], At[:, cb:cb + 128], ident16[:])
                        return ins
                    P.op("tensor", fnT, r=[Atb, B_id16], w=[ptb])
                    dstv = ATt[:, g4 * 4:g4 * 4 + nb, :]
                    srcv = pt_[:, 0:nb * 128].rearrange("p (j t) -> p j t", j=nb)
                    cnt["k"] += 1
                    if cnt["k"] % 2 == 0:
                        P.op("scalar", lambda h: h.copy(dstv, srcv), r=[ptb], w=[ATb])
                    else:
                        P.op("vector", lambda h: h.tensor_copy(dstv, srcv), r=[ptb], w=[ATb])
                U["ATt"], U["ATb"] = ATt, ATb

            def stD(U):
                hd, qb, L = U["hd"], U["qb"], U["L"]
                st, stb = U["st"], U["stb"]
                ATt, ATb = U["ATt"], U["ATb"]
                vt, vb_, ot, otb = hstate[hd]
                nblk = L // 128
                po_, pob = psO.next()

                def fnO(h):
                    for j in range(nblk):
                        ins = h.matmul(po_[:], ATt[:, j, :], vt[:, j, :], start=(j == 0), stop=(j == nblk - 1))
                    return ins
                P.op("tensor", fnO, r=[ATb, vb_], w=[pob])
                jt, jb = jr.next()
                P.op("scalar", lambda h: h.activation(out=jt[:], in_=po_[:], func=AF.Square, accum_out=st[:, 30:31]), r=[pob], w=[jb, stb])
                P.op("scalar", lambda h: h.activation(out=st[:, 31:32], in_=st[:, 30:31], func=AF.Sqrt, scale=1.0 / 128, bias=EPS_AP[:, 0:1]),
                     r=[stb, B_eps], w=[stb])
                P.op("vector", lambda h: h.reciprocal(st[:, 32:33], st[:, 31:32]), r=[stb], w=[stb])
                Ot, Otb = Or.next()
                P.op("vector", lambda h: h.scalar_tensor_tensor(Ot[:], po_[:], st[:, 32:33], sgt[:], ALU.mult, ALU.mult),
                     r=[pob, stb, B_sg], w=[Otb])
                py, pyb = psY.next()
                P.op("tensor", lambda h: h.transpose(py[:], Ot[:], ident[:]), r=[Otb, B_ident], w=[pyb])
                P.op("scalar", lambda h: h.copy(ot[:, qb * 128:(qb + 1) * 128], py[:]), r=[pyb], w=[otb])
                if qb == NT - 1:
                    P.dma("sync", yT_d[hd, :, :], ot[:], r=[otb], w=[B_yT[hd]])

            n = len(units)
            for s_ in range(n + 3):
                if 0 <= s_ - 3 < n:
                    stD(units[s_ - 3])
                if s_ < n:
                    stA(units[s_])
                if 0 <= s_ - 1 < n:
                    stB(units[s_ - 1])
                if 0 <= s_ - 2 < n:
                    stC(units[s_ - 2])
        P.barrier()

    L0, L1 = 0, 6144
    if upto >= 1 and lo_ph <= 1:
        proj_phase("p0", x_in, None, even_w_in, L0 + 1024, L0 + 0, [
            (0, 1024, "F", qk_d, B_qk, BF16, 0),
            (1024, 512, "T", v_d, B_v, BF16, 0),
            (1536, 1024, "F", xg_d, B_xg, F32, 0),
        ])
    if upto >= 2 and lo_ph <= 2:
        lru_phase()
    if upto >= 3 and lo_ph <= 3:
        sb_attn_phase()
    if upto >= 4 and lo_ph <= 4:
        outproj_phase("o0", x_in, None, xs_d[0], B_xs[0], even_w_out, L0 + 2048)
    if upto >= 5 and lo_ph <= 5:
        ffn_phase("f0", xs_d[0], B_xs[0], xs_d[1], B_xs[1], ffn_wg, ffn_wu, ffn_wd, 1, 2816, L0 + 4096, L0 + 3072, L0 + 5120)
    if upto >= 6 and lo_ph <= 6:
        proj_phase("p1", xs_d[1], B_xs[1], odd_w_in, L1 + 1024, L1 + 0, [
            (0, 2048, "F", qk_d, B_qk, BF16, 0),
            (2048, 1024, "T", v_d, B_v, BF16, 0),
        ])
    if upto >= 7 and lo_ph <= 7:
        diff_attn_phase()
    if upto >= 8 and lo_ph <= 8:
        outproj_phase("o1", xs_d[1], B_xs[1], xs_d[2], B_xs[2], odd_w_out, L1 + 2048)
    if upto >= 9:
        ffn_phase("f1", xs_d[2], B_xs[2], out_d, B_out, moe_wg, moe_wu, moe_wd, 8, 3584, L1 + 4096, L1 + 3072, L1 + 5120, final=True)
    else:
        zt = sb(ctx, "zt", [128, D])
        bz = Buf()
        P.op("vector", lambda h: h.memset(zt[:], 0.0), w=[bz])
        P.dma("sync", out_d[0:128, :], zt[:], r=[bz], w=[B_out[0]])
    P.barrier()
    ctx.close()
    return nc, P


def _bucket_table():
    n = np.arange(0, 256, dtype=np.int64)
    nf = np.maximum(n, 1).astype(np.float32)
    large = 16 + (np.log(nf / np.float32(16)) / np.float32(math.log(128 / 16)) * np.float32(16)).astype(np.int32)
    large = np.minimum(large, 31)
    return np.where(n < 16, n, large)


def make_in_maps(inp, names=None):
    f = lambda a: np.ascontiguousarray(np.asarray(a, dtype=np.float32))
    tq = np.arange(128)[:, None]
    sk = np.arange(128)[None, :]
    bt = _bucket_table()
    bk = np.zeros((128, 256), np.float32)
    bk[:, 0:128] = bt[128 + tq - sk]
    rel0 = tq - sk
    bk[:, 128:256] = np.where(rel0 >= 0, bt[np.maximum(rel0, 0)], -1)
    lru_vec = np.zeros((128, 4, 8), np.float32)
    cw = f(inp["lru_conv_w"])[0]
    for i in range(4):
        lru_vec[:, :, i] = cw[i].reshape(4, 128).T
    for j, kname in enumerate(("lru_conv_b", "lru_gate_a_b", "lru_gate_x_b", "lru_lambda")):
        lru_vec[:, :, 4 + j] = f(inp[kname])[0].reshape(4, 128).T
    shared = {
        "rel_bias": f(inp["rel_bias"]).reshape(1, 256),
        "ada_w": f(inp["ada_w"]),
        "ada_b": f(inp["ada_b"]).reshape(1, 12 * D),
        "ln_all": np.concatenate([f(inp["ln_mix"])[0], f(inp["ln_ffn"])[0], f(inp["ln_mix"])[1], f(inp["ln_ffn"])[1],
                                  f(inp["ln_final"])]).reshape(1, 5 * D),
        "even_w_in": f(inp["even_w_in"])[0],
        "even_w_out": f(inp["even_w_out"])[0],
        "lru_vec": lru_vec,
        "lru_ga": f(inp["lru_gate_a_w"])[0],
        "lru_gx": f(inp["lru_gate_x_w"])[0],
        "ffn_wg": f(inp["ffn_w_gate"]),
        "ffn_wu": f(inp["ffn_w_up"]),
        "ffn_wd": f(inp["ffn_w_down"]),
        "odd_w_in": f(inp["odd_w_in"])[0],
        "odd_w_out": f(inp["odd_w_out"])[0],
        "dl": np.concatenate([f(inp["diff_lambda_q1"])[0], f(inp["diff_lambda_k1"])[0], f(inp["diff_lambda_q2"])[0],
                              f(inp["diff_lambda_k2"])[0]]).reshape(1, 256),
        "subln": f(inp["diff_subln"]).reshape(1, 128),
        "router_w": f(inp["router_w"])[0],
        "router_b": f(inp["router_b"]).reshape(1, 8),
        "moe_wg": f(inp["moe_w_gate"])[0],
        "moe_wu": f(inp["moe_w_up"])[0],
        "moe_wd": f(inp["moe_w_down"])[0],
        "ident": np.eye(128, dtype=np.float32),
        "lmask": (sk < tq).astype(np.float32),
        "bk": bk,
        "negm": np.where(sk > tq, np.float32(-1e30), np.float32(0)).astype(np.float32),
    }
    x = f(inp["x"])
    cc = f(inp["c"])
    maps = []
    for b in range(8):
        m = dict(shared)
        m["x"] = x[b]
        m["cT"] = np.ascontiguousarray(cc[b].reshape(8, 128).T)
        if names is not None:
            m = {k: v for k, v in m.items() if k in names}
        maps.append(m)
    return maps


def kernel(**inputs):
    nc, _ = build()
    maps = make_in_maps(inputs)
    res = run_bass_kernel_spmd(nc, maps, core_ids=list(range(8)))
    return np.stack([np.asarray(r["out"], dtype=np.float32) for r in res.results], axis=0)
```

```python
import math
from contextlib import ExitStack
import numpy as np
import concourse.bass as bass
import concourse.mybir as mybir
from concourse.bass_utils import run_bass_kernel_spmd

F32 = mybir.dt.float32
BF16 = mybir.dt.bfloat16
AF = mybir.ActivationFunctionType
ALU = mybir.AluOpType
AX = mybir.AxisListType

S = 4096
D = 1024
NT = S // 128
EPS = 1e-6
LAMBDA_INIT = 0.8 - 0.6 * math.exp(-0.3 * 1)


class Buf:
    __slots__ = ("w", "r")

    def __init__(self):
        self.w = {}
        self.r = {}


def bufs(n):
    return [Buf() for _ in range(n)]


class Prog:
    CE = ("scalar", "vector", "gpsimd", "tensor")
    DQ = {"sync": 8, "scalar": 3, "gpsimd": 8}

    def __init__(self, nc, ctx):
        self.nc = nc
        self.h = {e: getattr(nc, e) for e in ("sync", "scalar", "vector", "gpsimd", "tensor")}
        self.semh = {}
        self.cnt = {}
        for e in self.CE:
            k = "e_" + e
            self.semh[k] = ctx.enter_context(nc.semaphore(k))
            self.cnt[k] = 0
        self.drr = {}
        for q, n in self.DQ.items():
            self.drr[q] = 0
            for i in range(n):
                k = "d_%s_%d" % (q, i)
                self.semh[k] = ctx.enter_context(nc.semaphore(k))
                self.cnt[k] = 0
        self.seen = {e: {} for e in self.h}
        self.ninst = 0

    def _waits(self, eng, r, w, extra=()):
        need = {}

        def add(d):
            for k, v in d.items():
                if need.get(k, 0) < v:
                    need[k] = v

        for b in r:
            add(b.w)
        for b in w:
            add(b.w)
            add(b.r)
        for k, v in extra:
            if need.get(k, 0) < v:
                need[k] = v
        seen = self.seen[eng]
        h = self.h[eng]
        for k, v in need.items():
            if seen.get(k, 0) < v:
                h.wait_ge(self.semh[k], v)
                seen[k] = v
                self.ninst += 1

    def _record(self, tok, r, w):
        k, v = tok
        for b in r:
            b.r[k] = v
        for b in w:
            if b.r:
                b.w = {k: v}
                b.r = {}
            else:
                b.w[k] = v

    def op(self, eng, fn, r=(), w=()):
        self._waits(eng, r, w)
        ins = fn(self.h[eng])
        k = "e_" + eng
        self.cnt[k] += 1
        ins.then_inc(self.semh[k], 1)
        self.ninst += 1
        self._record((k, self.cnt[k]), r, w)

    def dma(self, q, out, in_, r=(), w=(), **kw):
        i = self.drr[q]
        self.drr[q] = (i + 1) % self.DQ[q]
        k = "d_%s_%d" % (q, i)
        self._waits(q, r, w, extra=((k, self.cnt[k]),) if self.cnt[k] else ())
        ins = self.h[q].dma_start(out=out, in_=in_, **kw)
        self.cnt[k] += 16
        ins.then_inc(self.semh[k], 16)
        self.ninst += 1
        self._record((k, self.cnt[k]), r, w)

    def barrier(self, engines=("sync", "scalar", "vector", "gpsimd", "tensor")):
        for e in engines:
            seen = self.seen[e]
            for k, v in self.cnt.items():
                if v and seen.get(k, 0) < v:
                    self.h[e].wait_ge(self.semh[k], v)
                    seen[k] = v


class Ring:
    def __init__(self, tiles):
        self.t = tiles
        self.b = bufs(len(tiles))
        self.i = 0

    def next(self):
        i = self.i
        self.i = (i + 1) % len(self.t)
        return self.t[i], self.b[i]


NEED = {"even_w_in": 1, "lru_vec": 2, "lru_ga": 2, "lru_gx": 2, "lmask": 3, "even_w_out": 4, "ffn_wg": 5, "ffn_wu": 5,
        "ffn_wd": 5, "odd_w_in": 6, "rel_bias": 7, "dl": 7, "subln": 7, "bk": 7, "negm": 7, "odd_w_out": 8,
        "router_w": 9, "router_b": 9, "moe_wg": 9, "moe_wu": 9, "moe_wd": 9}


def build(dump=(), upto=99, moe_groups=4, moe_nexp=8, skip=(), lo_ph=1, RT=99):
    nc = bass.Bass("TRN2", target_bir_lowering=False)
    ctx = ExitStack()
    P = Prog(nc, ctx)
    P.in_names = []

    def din(name, shape, dt=F32):
        if NEED.get(name, 0) > upto or (0 < NEED.get(name, 0) < lo_ph and NEED.get(name, 0) != 9) or (moe_nexp == 0 and name.startswith("moe_w")):
            return None
        P.in_names.append(name)
        return nc.dram_tensor(name, list(shape), dt, kind="ExternalInput").ap()

    def dscr(name, shape, dt):
        return nc.dram_tensor(name, list(shape), dt, kind=("ExternalOutput" if name in dump else "Internal")).ap()

    x_in = din("x", [S, D])
    cT = din("cT", [128, 8])
    rel_bias = din("rel_bias", [1, 256])
    ada_w = din("ada_w", [2, D, 6 * D])
    ada_b = din("ada_b", [1, 12 * D])
    ln_all = din("ln_all", [1, 5 * D])
    even_w_in = din("even_w_in", [D, 2560])
    even_w_out = din("even_w_out", [D, D])
    lru_vec = din("lru_vec", [128, 4, 8])
    lru_ga = din("lru_ga", [8, 64, 64])
    lru_gx = din("lru_gx", [8, 64, 64])
    ffn_wg = din("ffn_wg", [1, D, 2816])
    ffn_wu = din("ffn_wu", [1, D, 2816])
    ffn_wd = din("ffn_wd", [1, 2816, D])
    odd_w_in = din("odd_w_in", [D, 3072])
    odd_w_out = din("odd_w_out", [D, D])
    dl = din("dl", [1, 256])
    subln = din("subln", [1, 128])
    router_w = din("router_w", [D, 8])
    router_b = din("router_b", [1, 8])
    moe_wg = din("moe_wg", [8, D, 3584])
    moe_wu = din("moe_wu", [8, D, 3584])
    moe_wd = din("moe_wd", [8, 3584, D])
    ident_d = din("ident", [128, 128])
    lmask_d = din("lmask", [128, 128])
    bk_d = din("bk", [128, 256])
    negm_d = din("negm", [128, 128])
    out_d = nc.dram_tensor("out", [S, D], F32, kind="ExternalOutput").ap()

    mod_d = dscr("mod_d", [1, 12 * D], F32)
    qk_d = dscr("qk_d", [16, 128, S], BF16)
    v_d = dscr("v_d", [S, 1024], BF16)
    xg_d = dscr("xg_d", [8, 128, S], F32)
    yT_d = dscr("yT_d", [8, 128, S], BF16)
    gates_dd = dscr("gates_d", [128, NT, 8], F32) if "gates_d" in dump else None
    lg_dd = dscr("lg_d", [128, NT, 32], F32) if "lg_d" in dump else None
    h32_dd = dscr("h32_d", [128, 8, 128], F32) if "lg_d" in dump else None
    xs_d = [dscr("xs%d_d" % i, [S, D], F32) for i in range(3)]

    B_mod = Buf()
    B_qk = bufs(16)
    B_v = bufs(NT)
    B_xg = bufs(8)
    B_yT = bufs(8)
    B_xs = [bufs(NT) for _ in range(3)]
    B_out = bufs(NT)

    def sb(c, name, shape, dt=F32):
        return c.enter_context(nc.sbuf_tensor(name, list(shape), dt))

    def ps(c, name, shape, dt=F32):
        return c.enter_context(nc.psum_tensor(name, list(shape), dt))

    ident = sb(ctx, "ident_sb", [128, 128])
    B_ident = Buf()
    P.dma("sync", ident[:], ident_d[:, :], w=[B_ident])
    ones1 = sb(ctx, "ones1", [1, 128])
    B_ones1 = Buf()
    P.op("vector", lambda h: h.memset(ones1[:], 1.0), w=[B_ones1])

    with ExitStack() as c0:
        cond = sb(c0, "cond", [128, 8])
        B_cond = Buf()
        P.dma("sync", cond[:], cT[:, :], w=[B_cond])
        P.op("scalar", lambda h: h.activation(out=cond[:], in_=cond[:], func=AF.Silu), r=[B_cond], w=[B_cond])
        modrow = sb(c0, "modrow", [1, 12 * D])
        B_modrow = Buf()
        brow = sb(c0, "brow", [1, 12 * D])
        B_brow = Buf()
        P.dma("sync", brow[:], ada_b[:, :], w=[B_brow])
        lnrow = sb(c0, "lnrow", [1, 5 * D])
        B_lnrow = Buf()
        P.dma("sync", lnrow[:], ln_all[:, :], w=[B_lnrow])
        wring = Ring([sb(c0, "adaw%d" % i, [128, 8, 512]) for i in range(2)])
        pring = Ring([ps(c0, "adaps%d" % i, [128, 512])[0:1, :] for i in range(2)])
        for l in range(2):
            wv = ada_w[l].rearrange("(kc p) n -> p kc n", p=128)
            for j in range(12):
                wt, wb = wring.next()
                P.dma("sync" if j % 2 == 0 else "scalar", wt[:], wv[:, :, j * 512:(j + 1) * 512], w=[wb])
                pt, pb = pring.next()

                def fn(h, wt=wt, pt=pt):
                    for kc in range(8):
                        ins = h.matmul(pt[:], cond[:, kc:kc + 1], wt[:, kc, :], start=(kc == 0), stop=(kc == 7))
                    return ins
                P.op("tensor", fn, r=[B_cond, wb], w=[pb])
                o = l * 6144 + j * 512
                P.op("vector", lambda h, pt=pt, o=o: h.tensor_tensor(modrow[:, o:o + 512], pt[:], brow[:, o:o + 512], ALU.add),
                     r=[pb, B_brow], w=[B_modrow])
        for l in range(2):
            for s_, (sc_off, ln_off) in enumerate(((1, 2 * l), (4, 2 * l + 1))):
                o = l * 6144 + sc_off * 1024
                lo = ln_off * 1024
                P.op("vector", lambda h, o=o, lo=lo: h.scalar_tensor_tensor(
                    modrow[:, o:o + 1024], modrow[:, o:o + 1024], 1.0, lnrow[:, lo:lo + 1024], ALU.add, ALU.mult),
                    r=[B_modrow, B_lnrow], w=[B_modrow])
        P.dma("sync", mod_d[:, :], modrow[:], r=[B_modrow], w=[B_mod])
    P.barrier()

    def load_bc(c, name, off, q="sync", n=1024, src=None, rb=None):
        t = sb(c, name, [128, n])
        b = Buf()
        s_ = (mod_d if src is None else src)[0:1, off:off + n]
        P.dma(q, t[:], s_.to_broadcast([128, n]), r=[B_mod if rb is None else rb], w=[b])
        return t, b

    class NormCtx:
        def __init__(self, c, tag, gm_off, sh_off, want32=False):
            self.gm, self.b_gm = load_bc(c, tag + "gm", gm_off)
            self.sh, self.b_sh = load_bc(c, tag + "sh", sh_off, q="scalar")
            self.xr = Ring([sb(c, tag + "xt%d" % i, [128, D]) for i in range(2)])
            self.hr = Ring([sb(c, tag + "ht%d" % i, [128, D]) for i in range(2)])
            self.jr = Ring([sb(c, tag + "junk%d" % i, [128, D]) for i in range(1)])
            self.sr = Ring([sb(c, tag + "st%d" % i, [128, 4]) for i in range(2)])
            self.pr = Ring([ps(c, tag + "tp%d" % i, [128, 512]) for i in range(2)])
            self.want32 = want32
            self.k = 0

        def run(self, xsrc_ap, xsrc_bufs, hT, hT_buf, col0, hT32=None, hT32_buf=None, keep_x=None):
            if keep_x is not None:
                xt, xb = keep_x
            else:
                xt, xb = self.xr.next()
            P.dma("sync", xt[:], xsrc_ap, r=xsrc_bufs, w=[xb])
            jt, jb = self.jr.next()
            st, stb = self.sr.next()
            P.op("vector", lambda h: h.memset(st[:], 0.0), w=[stb])
            P.op("scalar", lambda h: h.activation(out=jt[:], in_=xt[:], func=AF.Square, accum_out=st[:, 0:1]),
                 r=[xb], w=[jb, stb])
            P.op("scalar", lambda h: h.activation(out=st[:, 1:2], in_=st[:, 0:1], func=AF.Ln, scale=1.0 / D, bias=EPS_AP[:, 0:1]),
                 r=[stb, B_eps], w=[stb])
            P.op("scalar", lambda h: h.activation(out=st[:, 2:3], in_=st[:, 1:2], func=AF.Exp, scale=-0.5), r=[stb], w=[stb])
            ht, hb = self.hr.next()
            P.op("vector", lambda h: h.scalar_tensor_tensor(ht[:], xt[:], st[:, 2:3], self.gm[:], ALU.mult, ALU.mult),
                 r=[xb, stb, self.b_gm], w=[hb])
            P.op("vector", lambda h: h.tensor_tensor(ht[:], ht[:], self.sh[:], ALU.add), r=[hb, self.b_sh], w=[hb])
            for half in range(2):
                pt, pb = self.pr.next()

                def fn(h, pt=pt, half=half):
                    for j in range(4):
                        kc = half * 4 + j
                        ins = h.transpose(pt[:, j * 128:(j + 1) * 128], ht[:, kc * 128:(kc + 1) * 128], ident[:])
                    return ins
                P.op("tensor", fn, r=[hb, B_ident], w=[pb])
                dst = hT[:, half * 4:(half + 1) * 4, col0:col0 + 128]
                src = pt[:].rearrange("p (j t) -> p j t", j=4)
                eng = "scalar" if (self.k % 2 == 0) else "vector"
                self.k += 1
                if hT32 is not None:
                    dst2 = hT32[:, half * 4:(half + 1) * 4, :]
                    P.op("vector", lambda h: h.tensor_copy(dst2, src), r=[pb], w=[hT32_buf])
                    P.op("scalar", lambda h: h.copy(dst, dst2), r=[hT32_buf], w=[hT_buf])
                elif eng == "scalar":
                    P.op("scalar", lambda h: h.copy(dst, src), r=[pb], w=[hT_buf])
                else:
                    P.op("vector", lambda h: h.tensor_copy(dst, src), r=[pb], w=[hT_buf])
            return xt, xb

    EPS_AP = sb(ctx, "eps_ap", [128, 1])
    B_eps = Buf()
    P.op("vector", lambda h: h.memset(EPS_AP[:], EPS), w=[B_eps])

    def proj_phase(tag, xsrc, xbufs, w_in, gm_off, sh_off, specs):
        with ExitStack() as c:
            hT = sb(c, tag + "hT", [128, 8, S], BF16)
            B_hT = bufs(NT)
            with ExitStack() as c1:
                ncx = NormCtx(c1, tag + "n", gm_off, sh_off)
                for t in range(NT):
                    ncx.run(xsrc[t * 128:(t + 1) * 128, :], [xbufs[t]] if xbufs else [], hT, B_hT[t], t * 128)
            P.barrier()
            wv = w_in.rearrange("(kc p) n -> p kc n", p=128)
            wr = Ring([sb(c, tag + "w%d" % i, [128, 8, 512], BF16) for i in range(2)])
            pr = Ring([ps(c, tag + "pp%d" % i, [128, 512]) for i in range(4)])
            stF32 = Ring([sb(c, tag + "sf%d" % i, [128, S], F32) for i in range(2)])
            stF16 = Ring([sb(c, tag + "sh%d" % i, [128, S], BF16) for i in range(2)])
            stT = Ring([sb(c, tag + "stT%d" % i, [128, 512], BF16) for i in range(3)])
            k = 0
            for (col0, ncols, mode, dst, dbufs, dt, chunk0) in specs:
                for c512 in range(ncols // 512):
                    wt, wb = wr.next()
                    P.dma("gpsimd", wt[:], wv[:, :, col0 + c512 * 512: col0 + (c512 + 1) * 512], w=[wb])
                    if mode == "F":
                        for fc in range(4):
                            stg, sgb = (stF32 if dt == F32 else stF16).next()
                            for nt in range(8):
                                pt, pb = pr.next()

                                def fn(h, pt=pt, wt=wt, fc=fc, nt=nt):
                                    for kc in range(8):
                                        ins = h.matmul(pt[:], wt[:, kc, fc * 128:(fc + 1) * 128],
                                                       hT[:, kc, nt * 512:(nt + 1) * 512], start=(kc == 0), stop=(kc == 7))
                                    return ins
                                P.op("tensor", fn, r=[wb] + B_hT[nt * 4:(nt + 1) * 4], w=[pb])
                                k += 1
                                if k % 2 == 0:
                                    P.op("scalar", lambda h, pt=pt, stg=stg, nt=nt: h.copy(stg[:, nt * 512:(nt + 1) * 512], pt[:]),
                                         r=[pb], w=[sgb])
                                else:
                                    P.op("vector", lambda h, pt=pt, stg=stg, nt=nt: h.tensor_copy(stg[:, nt * 512:(nt + 1) * 512], pt[:]),
                                         r=[pb], w=[sgb])
                            ch = chunk0 + c512 * 4 + fc
                            P.dma("sync", dst[ch, :, :], stg[:], r=[sgb], w=[dbufs[ch]])
                    else:
                        for t in range(NT):
                            pt, pb = pr.next()

                            def fn(h, pt=pt, wt=wt, t=t):
                                for kc in range(8):
                                    ins = h.matmul(pt[:], hT[:, kc, t * 128:(t + 1) * 128], wt[:, kc, :],
                                                   start=(kc == 0), stop=(kc == 7))
                                return ins
                            P.op("tensor", fn, r=[wb, B_hT[t]], w=[pb])
                            stg, sgb = stT.next()
                            k += 1
                            if k % 2 == 0:
                                P.op("scalar", lambda h, pt=pt, stg=stg: h.copy(stg[:], pt[:]), r=[pb], w=[sgb])
                            else:
                                P.op("vector", lambda h, pt=pt, stg=stg: h.tensor_copy(stg[:], pt[:]), r=[pb], w=[sgb])
                            P.dma("sync", dst[t * 128:(t + 1) * 128, c512 * 512:(c512 + 1) * 512], stg[:], r=[sgb], w=[dbufs[t]])
        P.barrier()

    def outproj_phase(tag, xsrc, xbufs, xdst, xdbufs, w_out, g_off):
        with ExitStack() as c:
            gbc, b_g = load_bc(c, tag + "g", g_off)
            wv = w_out.rearrange("(kc p) n -> p kc n", p=128)
            wt = sb(c, tag + "w", [128, 8, D], BF16)
            B_w = Buf()
            P.dma("gpsimd", wt[:, :, 0:512], wv[:, :, 0:512], w=[B_w])
            P.dma("gpsimd", wt[:, :, 512:1024], wv[:, :, 512:1024], w=[B_w])
            yr = Ring([sb(c, tag + "y%d" % i, [128, 8, 512], BF16) for i in range(2)])
            xr = Ring([sb(c, tag + "x%d" % i, [128, D]) for i in range(3)])
            pr = Ring([ps(c, tag + "p%d" % i, [128, 512]) for i in range(4)])
            tr = Ring([sb(c, tag + "t%d" % i, [128, 512]) for i in range(2)])
            for g4 in range(8):
                yt, yb = yr.next()
                P.dma("sync", yt[:], yT_d[:, :, g4 * 512:(g4 + 1) * 512].rearrange("k p t -> p k t"), r=B_yT, w=[yb])
                for tt in range(4):
                    t = g4 * 4 + tt
                    xt, xb = xr.next()
                    P.dma("scalar", xt[:], xsrc[t * 128:(t + 1) * 128, :], r=[xbufs[t]] if xbufs else [], w=[xb])
                    for half in range(2):
                        pt, pb = pr.next()

                        def fn(h, pt=pt, yt=yt, tt=tt, half=half):
                            for kc in range(8):
                                ins = h.matmul(pt[:], yt[:, kc, tt * 128:(tt + 1) * 128], wt[:, kc, half * 512:(half + 1) * 512],
                                               start=(kc == 0), stop=(kc == 7))
                            return ins
                        P.op("tensor", fn, r=[yb, B_w], w=[pb])
                        sl = slice(half * 512, (half + 1) * 512)
                        tt_, ttb = tr.next()
                        P.op("vector", lambda h, pt=pt, xt=xt, sl=sl: h.tensor_tensor(tt_[:], pt[:], gbc[:, sl], ALU.mult),
                             r=[pb, b_g], w=[ttb])
                        P.op("gpsimd", lambda h, pt=pt, xt=xt, sl=sl: h.tensor_tensor(xt[:, sl], xt[:, sl], tt_[:], ALU.add),
                             r=[ttb, xb], w=[xb])
                    P.dma("sync", xdst[t * 128:(t + 1) * 128, :], xt[:], r=[xb], w=[xdbufs[t]])
        P.barrier()

    def ffn_phase(tag, xsrc, xbufs, xdst, xdbufs, wg, wu, wd, n_exp, dff, gm_off, sh_off, g_off, final=False):
        G = 1024
        ftiles = []
        f0 = 0
        while f0 < dff:
            wdt = min(512, dff - f0)
            ftiles.append((f0, wdt))
            f0 += wdt
        subgroups = [ftiles[i:i + 2] for i in range(0, len(ftiles), 2)]
        with ExitStack() as c:
            gbc, b_g = load_bc(c, tag + "g", g_off)
            if final:
                lnf, b_lnf = load_bc(c, tag + "lnf", 4 * D, src=ln_all, rb=Buf())
            ncx = NormCtx(c, tag + "n", gm_off, sh_off)
            hT = sb(c, tag + "hT", [128, 8, G], BF16)
            ysum = sb(c, tag + "ysum", [128, 8, D])
            B_ys = bufs(8)
            actT = sb(c, tag + "actT", [128, 8, G], BF16)
            wgr = Ring([sb(c, tag + "wg%d" % i, [128, 8, 512], BF16) for i in range(2)])
            wur = Ring([sb(c, tag + "wu%d" % i, [128, 8, 512], BF16) for i in range(2)])
            wdr = Ring([sb(c, tag + "wd%d" % i, [128, 8, D], BF16) for i in range(2)])
            sgr = Ring([sb(c, tag + "sg%d" % i, [128, 512]) for i in range(3)])
            psg = Ring([ps(c, tag + "pg%d" % i, [128, 512]) for i in range(2)])
            psu = Ring([ps(c, tag + "pu%d" % i, [128, 512]) for i in range(2)])
            psd = Ring([ps(c, tag + "pd%d" % i, [128, 512]) for i in range(2)])
            if n_exp > 1:
                gates = sb(c, tag + "gates", [128, 8, 8])
                hT32r = Ring([sb(c, tag + "h32_%d" % i, [128, 8, 128]) for i in range(2)])
                rw = sb(c, tag + "rw", [128, 8, 8])
                B_rw = Buf()
                P.dma("sync", rw[:], router_w.rearrange("(kc p) e -> p kc e", p=128), w=[B_rw])
                rbb, b_rbb = load_bc(c, tag + "rb", 0, n=8, src=router_b, rb=Buf())
                lgr = Ring([sb(c, tag + "lg%d" % i, [128, 32]) for i in range(2)])
            B_hT = bufs(8)
            B_act = Buf()
            B_gates = bufs(8)
            for g in range(min(S // G, moe_groups if n_exp > 1 else 99)):
                for tt in range(8):
                    t = g * 8 + tt
                    if n_exp > 1 and 'h32' not in skip:
                        h32, h32b = hT32r.next()
                    else:
                        h32, h32b = None, None
                    ncx.run(xsrc[t * 128:(t + 1) * 128, :], [xbufs[t]] if xbufs else [], hT, B_hT[tt], tt * 128,
                            hT32=h32, hT32_buf=h32b)
                    if n_exp > 1 and 'router' not in skip:
                        pt, pb = psd.next()
                        _rt = [0]

                        def POP(*a, **k):
                            _rt[0] += 1
                            if _rt[0] <= RT:
                                P.op(*a, **k)

                        def fn(h, pt=pt, h32=h32):
                            for kc in range(8):
                                ins = h.matmul(pt[:, 0:8], h32[:, kc, :], rw[:, kc, :], start=(kc == 0), stop=(kc == 7))
                            return ins
                        POP("tensor", fn, r=[h32b, B_rw], w=[pb])
                        lg, lgb = lgr.next()
                        POP("vector", lambda h: h.tensor_tensor(lg[:, 0:8], pt[:, 0:8], rbb[:], ALU.add), r=[pb, b_rbb], w=[lgb])
                        POP("vector", lambda h: h.max(lg[:, 8:16], lg[:, 0:8]), r=[lgb], w=[lgb])
                        POP("vector", lambda h: h.tensor_scalar(lg[:, 24:25], lg[:, 8:9], -1.0, None, ALU.mult), r=[lgb], w=[lgb])
                        POP("scalar", lambda h: h.activation(out=lg[:, 16:24], in_=lg[:, 0:8], func=AF.Exp, bias=lg[:, 24:25]),
                             r=[lgb], w=[lgb])
                        POP("vector", lambda h: h.scalar_tensor_tensor(lg[:, 16:24], lg[:, 0:8], lg[:, 9:10], lg[:, 16:24],
                                                                       ALU.is_ge, ALU.mult), r=[lgb], w=[lgb])
                        POP("vector", lambda h: h.tensor_reduce(lg[:, 25:26], lg[:, 16:24], AX.X, ALU.add), r=[lgb], w=[lgb])
                        POP("vector", lambda h: h.reciprocal(lg[:, 26:27], lg[:, 25:26]), r=[lgb], w=[lgb])
                        POP("vector", lambda h: h.tensor_scalar(gates[:, tt, :], lg[:, 16:24], lg[:, 26:27], None, ALU.mult),
                             r=[lgb], w=[B_gates[tt]])
                        if "lg_d" in dump:
                            P.dma("sync", lg_dd[:, t, :], lg[:], r=[lgb], w=[Buf()])
                            if t == 0:
                                P.dma("sync", h32_dd[:, :, :], h32[:], r=[h32b], w=[Buf()])
                if n_exp > 1 and "lg_d" in dump and 'router' not in skip:
                    pass
                if n_exp > 1 and "gates_d" in dump:
                    P.dma("sync", gates_dd[:, g * 8:(g + 1) * 8, :], gates[:], r=B_gates, w=[Buf()])
                for e in range(n_exp if n_exp == 1 else moe_nexp):
                    wgv = wg[e].rearrange("(kc p) f -> p kc f", p=128)
                    wuv = wu[e].rearrange("(kc p) f -> p kc f", p=128)
                    for si, sg_ in enumerate(subgroups):
                        nch = sum(w_ // 128 for _, w_ in sg_)
                        wdt_, wdb = wdr.next()
                        ch = 0
                        for (f0, fw) in sg_:
                            P.dma("gpsimd", wdt_[:, ch:ch + fw // 128, :],
                                  wd[e, f0:f0 + fw, :].rearrange("(fc p) d -> p fc d", p=128), w=[wdb])
                            ch += fw // 128
                        ch = 0
                        for (f0, fw) in sg_:
                            wgt, wgb = wgr.next()
                            wut, wub = wur.next()
                            P.dma("gpsimd", wgt[:, :, 0:fw], wgv[:, :, f0:f0 + fw], w=[wgb])
                            P.dma("gpsimd", wut[:, :, 0:fw], wuv[:, :, f0:f0 + fw], w=[wub])
                            for fc in range(fw // 128):
                                for nt in range(G // 512):
                                    pg, pgb = psg.next()
                                    pu, pub = psu.next()

                                    def fn(h, pg=pg, pu=pu, wgt=wgt, wut=wut, fc=fc, nt=nt):
                                        for kc in range(8):
                                            h.matmul(pg[:], wgt[:, kc, fc * 128:(fc + 1) * 128], hT[:, kc, nt * 512:(nt + 1) * 512],
                                                     start=(kc == 0), stop=(kc == 7))
                                        for kc in range(8):
                                            ins = h.matmul(pu[:], wut[:, kc, fc * 128:(fc + 1) * 128], hT[:, kc, nt * 512:(nt + 1) * 512],
                                                           start=(kc == 0), stop=(kc == 7))
                                        return ins
                                    P.op("tensor", fn, r=[wgb, wub] + B_hT[nt * 4:(nt + 1) * 4], w=[pgb, pub])
                                    sgt, sgb = sgr.next()
                                    P.op("scalar", lambda h, pg=pg, sgt=sgt: h.activation(out=sgt[:], in_=pg[:], func=AF.Silu),
                                         r=[pgb], w=[sgb])
                                    P.op("vector", lambda h, pu=pu, sgt=sgt, ch=ch, fc=fc, nt=nt: h.tensor_tensor(
                                        actT[:, ch + fc, nt * 512:(nt + 1) * 512], sgt[:], pu[:], ALU.mult),
                                        r=[sgb, pub], w=[B_act])
                            ch += fw // 128
                        for tt in range(8):
                            for half in range(2):
                                pd, pdb = psd.next()

                                def fn(h, pd=pd, tt=tt, half=half, wdt_=wdt_, nch=nch):
                                    for cc in range(nch):
                                        ins = h.matmul(pd[:], actT[:, cc, tt * 128:(tt + 1) * 128], wdt_[:, cc, half * 512:(half + 1) * 512],
                                                       start=(cc == 0), stop=(cc == nch - 1))
                                    return ins
                                P.op("tensor", fn, r=[B_act, wdb], w=[pdb])
                                ysl = ysum[:, tt, half * 512:(half + 1) * 512]
                                first = (e == 0 and si == 0)
                                if n_exp > 1:
                                    gap = gates[:, tt, e:e + 1]
                                    if first:
                                        P.op("vector", lambda h, pd=pd, ysl=ysl, gap=gap: h.tensor_scalar(ysl, pd[:], gap, None, ALU.mult),
                                             r=[pdb, B_gates[tt]], w=[B_ys[tt]])
                                    else:
                                        P.op("vector", lambda h, pd=pd, ysl=ysl, gap=gap: h.scalar_tensor_tensor(
                                            ysl, pd[:], gap, ysl, ALU.mult, ALU.add), r=[pdb, B_gates[tt], B_ys[tt]], w=[B_ys[tt]])
                                else:
                                    if first:
                                        P.op("vector", lambda h, pd=pd, ysl=ysl: h.tensor_copy(ysl, pd[:]), r=[pdb], w=[B_ys[tt]])
                                    else:
                                        P.op("vector", lambda h, pd=pd, ysl=ysl: h.tensor_tensor(ysl, ysl, pd[:], ALU.add),
                                             r=[pdb, B_ys[tt]], w=[B_ys[tt]])
                for tt in range(8):
                    t = g * 8 + tt
                    xt, xb = ncx.xr.next()
                    P.dma("sync", xt[:], xsrc[t * 128:(t + 1) * 128, :], r=[xbufs[t]] if xbufs else [], w=[xb])
                    P.op("vector", lambda h: h.tensor_tensor(ysum[:, tt, :], ysum[:, tt, :], gbc[:], ALU.mult),
                         r=[B_ys[tt], b_g], w=[B_ys[tt]])
                    P.op("vector", lambda h: h.tensor_tensor(xt[:], xt[:], ysum[:, tt, :], ALU.add), r=[B_ys[tt], xb], w=[xb])
                    if final and 'final' not in skip:
                        jt, jb = ncx.jr.next()
                        st, stb = ncx.sr.next()
                        P.op("vector", lambda h: h.memset(st[:], 0.0), w=[stb])
                        P.op("scalar", lambda h: h.activation(out=jt[:], in_=xt[:], func=AF.Square, accum_out=st[:, 0:1]),
                             r=[xb], w=[jb, stb])
                        P.op("scalar", lambda h: h.activation(out=st[:, 1:2], in_=st[:, 0:1], func=AF.Sqrt, scale=1.0 / D,
                                                              bias=EPS_AP[:, 0:1]), r=[stb, B_eps], w=[stb])
                        P.op("vector", lambda h: h.reciprocal(st[:, 2:3], st[:, 1:2]), r=[stb], w=[stb])
                        P.op("vector", lambda h: h.scalar_tensor_tensor(xt[:], xt[:], st[:, 2:3], lnf[:], ALU.mult, ALU.mult),
                             r=[xb, stb, b_lnf], w=[xb])
                    P.dma("sync", xdst[t * 128:(t + 1) * 128, :], xt[:], r=[xb], w=[xdbufs[t]])
        P.barrier()

    def lru_phase():
        with ExitStack() as c:
            lv = sb(c, "lv", [128, 4, 8])
            B_lv = Buf()
            P.dma("sync", lv[:], lru_vec[:, :, :], w=[B_lv])
            c8 = sb(c, "c8", [128, 4, 4])
            B_c8 = Buf()
            P.op("scalar", lambda h: h.activation(out=c8[:, :, 2], in_=lv[:, :, 7], func=AF.Exp, scale=-1.0), r=[B_lv], w=[B_c8])
            P.op("scalar", lambda h: h.activation(out=c8[:, :, 3], in_=c8[:, :, 2], func=AF.Ln, bias=1.0), r=[B_c8], w=[B_c8])
            P.op("vector", lambda h: h.tensor_scalar(c8[:, :, 0], c8[:, :, 3], -8.0, None, ALU.mult), r=[B_c8], w=[B_c8])
            P.op("vector", lambda h: h.tensor_scalar(c8[:, :, 1], c8[:, :, 3], -16.0, None, ALU.mult), r=[B_c8], w=[B_c8])
            gA = sb(c, "gA", [128, 4, 128], BF16)
            gX = sb(c, "gX", [128, 4, 128], BF16)
            B_gw = Buf()
            P.op("vector", lambda h: h.memset(gA[:], 0.0), w=[B_gw])
            P.op("vector", lambda h: h.memset(gX[:], 0.0), w=[B_gw])
            for cc in range(4):
                for j in range(2):
                    P.dma("gpsimd", gA[j * 64:(j + 1) * 64, cc, j * 64:(j + 1) * 64], lru_ga[2 * cc + j, :, :], w=[B_gw])
                    P.dma("gpsimd", gX[j * 64:(j + 1) * 64, cc, j * 64:(j + 1) * 64], lru_gx[2 * cc + j, :, :], w=[B_gw])
            XBr = Ring([sb(c, "lxb%d" % i, [128, S + 4]) for i in range(2)])
            GBr = Ring([sb(c, "lgb%d" % i, [128, S]) for i in range(2)])
            YBr = Ring([sb(c, "lyb%d" % i, [128, S], BF16) for i in range(2)])
            mk = lambda nm, n, dt=F32: Ring([sb(c, "%s%d" % (nm, i), [128, 512], dt) for i in range(n)])
            XCr, XCbr, Rr, Ir, A2r, Ur, Hr, GGr = mk("lxc", 2), mk("lxcb", 2, BF16), mk("lr", 2), mk("li", 2), mk("la2", 2), \
                mk("lu", 2), mk("lh", 3), mk("lgg", 2)
            psr = Ring([ps(c, "lpr%d" % i, [128, 512]) for i in range(2)])
            psi = Ring([ps(c, "lpi%d" % i, [128, 512]) for i in range(2)])
            for cc in range(4):
                xbt, xbb = XBr.next()
                gbt, gbb = GBr.next()
                ybt, ybb = YBr.next()
                P.op("vector", lambda h: h.memset(xbt[:, 0:4], 0.0), w=[xbb])
                P.dma("sync", xbt[:, 4:S + 4], xg_d[cc, :, :], r=[B_xg[cc]], w=[xbb])
                P.dma("scalar", gbt[:], xg_d[4 + cc, :, :], r=[B_xg[4 + cc]], w=[gbb])
                hprev = None
                for nt in range(8):
                    o = nt * 512
                    xc, xcb = XCr.next()
                    P.op("vector", lambda h: h.tensor_scalar(xc[:], xbt[:, o + 1:o + 513], lv[:, cc, 0:1], lv[:, cc, 4:5], ALU.mult, ALU.add),
                         r=[xbb, B_lv], w=[xcb])
                    for i in range(1, 4):
                        P.op("vector", lambda h, i=i: h.scalar_tensor_tensor(xc[:], xbt[:, o + 1 + i:o + 513 + i], lv[:, cc, i:i + 1], xc[:],
                                                                             ALU.mult, ALU.add), r=[xbb, B_lv, xcb], w=[xcb])
                    xcbf, xcbfb = XCbr.next()
                    P.op("gpsimd", lambda h: h.tensor_copy(xcbf[:], xc[:]), r=[xcb], w=[xcbfb])
                    pr_, prb = psr.next()
                    pi_, pib = psi.next()

                    def fn(h):
                        h.matmul(pr_[:], gA[:, cc, :], xcbf[:], start=True, stop=True)
                        return h.matmul(pi_[:], gX[:, cc, :], xcbf[:], start=True, stop=True)
                    P.op("tensor", fn, r=[B_gw, xcbfb], w=[prb, pib])
                    rt, rb_ = Rr.next()
                    it, ib_ = Ir.next()
                    P.op("scalar", lambda h: h.activation(out=rt[:], in_=pr_[:], func=AF.Sigmoid, bias=lv[:, cc, 5:6]), r=[prb, B_lv], w=[rb_])
                    P.op("scalar", lambda h: h.activation(out=it[:], in_=pi_[:], func=AF.Sigmoid, bias=lv[:, cc, 6:7]), r=[pib, B_lv], w=[ib_])
                    a2, a2b = A2r.next()
                    P.op("scalar", lambda h: h.activation(out=a2[:], in_=rt[:], func=AF.Exp, scale=c8[:, cc, 1:2]), r=[rb_, B_c8], w=[a2b])
                    P.op("scalar", lambda h: h.activation(out=rt[:], in_=rt[:], func=AF.Exp, scale=c8[:, cc, 0:1]), r=[rb_, B_c8], w=[rb_])
                    P.op("scalar", lambda h: h.activation(out=a2[:], in_=a2[:], func=AF.Sqrt, scale=-1.0, bias=1.0), r=[a2b], w=[a2b])
                    gg, ggb = GGr.next()
                    P.op("scalar", lambda h: h.activation(out=gg[:], in_=gbt[:, o:o + 512], func=AF.Gelu), r=[gbb], w=[ggb])
                    ut, ub_ = Ur.next()
                    P.op("vector", lambda h: h.tensor_tensor(ut[:], a2[:], it[:], ALU.mult), r=[a2b, ib_], w=[ub_])
                    P.op("vector", lambda h: h.tensor_tensor(ut[:], ut[:], xc[:], ALU.mult), r=[ub_, xcb], w=[ub_])
                    ht_, hb_ = Hr.next()
                    if hprev is None:
                        P.op("vector", lambda h: h.tensor_tensor_scan(ht_[:], rt[:], ut[:], 0.0, ALU.mult, ALU.add), r=[rb_, ub_], w=[hb_])
                    else:
                        hp, hpb = hprev
                        P.op("vector", lambda h: h.tensor_tensor_scan(ht_[:], rt[:], ut[:], hp[:, 511:512], ALU.mult, ALU.add),
                             r=[rb_, ub_, hpb], w=[hb_])
                    hprev = (ht_, hb_)
                    P.op("vector", lambda h: h.tensor_tensor(ybt[:, o:o + 512], ht_[:], gg[:], ALU.mult), r=[hb_, ggb], w=[ybb])
                P.dma("sync", yT_d[4 + cc, :, :], ybt[:], r=[ybb], w=[B_yT[4 + cc]])
        P.barrier()

    def sb_attn_phase():
        with ExitStack() as c:
            lmask = sb(c, "lmask_sb", [128, 128])
            B_lm = Buf()
            P.dma("sync", lmask[:], lmask_d[:, :], w=[B_lm])
            lmask16 = sb(c, "lmask16", [128, 128], BF16)
            P.op("vector", lambda h: h.tensor_copy(lmask16[:], lmask[:]), r=[B_lm], w=[B_lm])
            onesw = sb(c, "onesw", [128, 512])
            B_on = Buf()
            P.op("vector", lambda h: h.memset(onesw[:], 1.0), w=[B_on])
            yaT = sb(c, "yaT", [128, 4, S], BF16)
            B_yaT = Buf()
            QTr = Ring([sb(c, "sq%d" % i, [64, S], BF16) for i in range(2)])
            KTr = Ring([sb(c, "sk%d" % i, [64, S], BF16) for i in range(2)])
            Vr = Ring([sb(c, "sv%d" % i, [128, NT, 64], BF16) for i in range(2)])
            mk = lambda nm, n, dt=F32, w_=512: Ring([sb(c, "%s%d" % (nm, i), [128, w_], dt) for i in range(n)])
            Er, SPr, PFr, T2r, Wr, WTr, NCr = mk("sE", 3), mk("sSP", 5), mk("sPF", 5), mk("sT2", 4), mk("sW", 4, BF16), \
                mk("sWT", 4, BF16), mk("sNC", 14, F32, 2)
            YA = sb(c, "sYA", [128, NT, 512])
            B_YA = bufs(NT)
            tiles = []
            for hd in range(8):
                for qb in range(NT):
                    L = (qb + 1) * 128
                    nkt = (L + 511) // 512
                    for kt in range(nkt - 1, -1, -1):
                        tiles.append(dict(hd=hd, qb=qb, kt=kt, W=min(512, L - kt * 512), diag=(kt == nkt - 1),
                                          first=(kt == nkt - 1), last=(kt == 0), nblk=L // 128))
            heads = {}

            def load_head(hd):
                if hd in heads or hd > 7:
                    return
                qt, qb_ = QTr.next()
                kt_, kb_ = KTr.next()
                vt, vb_ = Vr.next()
                po = (hd % 2) * 64
                P.dma("sync", qt[:], qk_d[hd // 2, po:po + 64, :], r=[B_qk[hd // 2]], w=[qb_])
                P.dma("scalar", kt_[:], qk_d[4 + hd // 2, po:po + 64, :], r=[B_qk[4 + hd // 2]], w=[kb_])
                P.dma("sync", vt[:], v_d[:, hd * 64:(hd + 1) * 64].rearrange("(b p) d -> p b d", p=128), r=B_v, w=[vb_])
                heads[hd] = (qt, qb_, kt_, kb_, vt, vb_)

            with ExitStack() as c2:
                psZ = Ring([ps(c2, "spz%d" % i, [128, 512]) for i in range(4)])
                psT = Ring([ps(c2, "spt%d" % i, [128, 1024], BF16)[:, 0:512] for i in range(3)])
                psO = Ring([ps(c2, "spo%d" % i, [128, 512])[:, 0:64] for i in range(1)])
                st = {"nc": None, "po": None, "blk": 0}

                def stA(T):
                    hd, qb, kt, W = T["hd"], T["qb"], T["kt"], T["W"]
                    if hd not in heads:
                        load_head(hd)
                    if T["first"] and qb == 0:
                        load_head(hd + 1)
                    qt, qb_, kt_, kb_, vt, vb_ = heads[hd]
                    pz, pzb = psZ.next()
                    P.op("tensor", lambda h: h.matmul(pz[:, 0:W], qt[:, qb * 128:(qb + 1) * 128], kt_[:, kt * 512:kt * 512 + W],
                                                      start=True, stop=True), r=[qb_, kb_], w=[pzb])
                    et, eb = Er.next()
                    spt, spb = SPr.next()
                    P.op("scalar", lambda h: h.activation(out=et[:, 0:W], in_=pz[:, 0:W], func=AF.Exp, scale=0.125), r=[pzb], w=[eb])
                    P.op("scalar", lambda h: h.activation(out=spt[:, 0:W], in_=et[:, 0:W], func=AF.Ln, bias=1.0), r=[eb], w=[spb])
                    T["pz"], T["pzb"], T["sp"], T["spb"] = pz, pzb, spt, spb

                def stB(T):
                    W = T["W"]
                    pz, pzb, spt, spb = T["pz"], T["pzb"], T["sp"], T["spb"]
                    if T["diag"]:
                        P.op("vector", lambda h: h.tensor_tensor(spt[:, W - 128:W], spt[:, W - 128:W], lmask[:], ALU.mult),
                             r=[spb, B_lm], w=[spb])
                    pf, pfb = PFr.next()
                    P.op("vector", lambda h: h.tensor_tensor_scan(pf[:, 0:W], onesw[:, 0:W], spt[:, 0:W], 0.0, ALU.mult, ALU.add),
                         r=[spb, B_on], w=[pfb])
                    nct2, ncb2 = NCr.next()
                    if T["first"]:
                        P.op("vector", lambda h: h.tensor_scalar(nct2[:, 0:1], pf[:, W - 1:W], -1.0, None, ALU.mult), r=[pfb], w=[ncb2])
                    else:
                        nct, ncb = st["nc"]
                        P.op("vector", lambda h: h.tensor_tensor(nct2[:, 0:1], nct[:, 0:1], pf[:, W - 1:W], ALU.subtract),
                             r=[ncb, pfb], w=[ncb2])
                    st["nc"] = (nct2, ncb2)
                    P.op("gpsimd", lambda h: h.tensor_tensor(pf[:, 0:W], pf[:, 0:W], spt[:, 0:W], ALU.subtract), r=[pfb, spb], w=[pfb])
                    T["pf"], T["pfb"], T["nc"] = pf, pfb, (nct2, ncb2)

                def stB2(T):
                    W = T["W"]
                    pz, pzb, pf, pfb = T["pz"], T["pzb"], T["pf"], T["pfb"]
                    nct2, ncb2 = T["nc"]
                    t2, t2b = T2r.next()
                    P.op("vector", lambda h: h.scalar_tensor_tensor(t2[:, 0:W], pz[:, 0:W], 0.125, pf[:, 0:W], ALU.mult, ALU.add),
                         r=[pzb, pfb], w=[t2b])
                    T["t2"], T["t2b"], T["nc"] = t2, t2b, (nct2, ncb2)

                def stC(T):
                    hd, qb, kt, W = T["hd"], T["qb"], T["kt"], T["W"]
                    qt, qb_, kt_, kb_, vt, vb_ = heads[hd]
                    t2, t2b = T["t2"], T["t2b"]
                    nct, ncb = T["nc"]
                    wt_, wb_ = Wr.next()
                    P.op("scalar", lambda h: h.activation(out=wt_[:, 0:W], in_=t2[:, 0:W], func=AF.Exp, bias=nct[:, 0:1]),
                         r=[t2b, ncb], w=[wb_])
                    if T["diag"]:
                        P.op("gpsimd", lambda h: h.tensor_tensor(wt_[:, W - 128:W], wt_[:, W - 128:W], lmask16[:], ALU.mult),
                             r=[wb_, B_lm], w=[wb_])
                    nb = W // 128
                    pt_, ptb = psT.next()

                    def fnT(h):
                        for j in range(nb):
                            ins = h.transpose(pt_[:, j * 128:(j + 1) * 128], wt_[:, j * 128:(j + 1) * 128], ident16[:])
                        return ins
                    P.op("tensor", fnT, r=[wb_, B_id16], w=[ptb])
                    T["pt"], T["ptb"], T["nb"] = pt_, ptb, nb

                def stD(T):
                    hd, qb, kt, W = T["hd"], T["qb"], T["kt"], T["W"]
                    qt, qb_, kt_, kb_, vt, vb_ = heads[hd]
                    pt_, ptb, nb = T["pt"], T["ptb"], T["nb"]
                    wT, wTb = WTr.next()
                    P.op("vector", lambda h: h.tensor_copy(wT[:, 0:W], pt_[:, 0:W]), r=[ptb], w=[wTb])
                    T["wT"], T["wTb"] = wT, wTb

                def stE(T):
                    hd, qb, kt, W = T["hd"], T["qb"], T["kt"], T["W"]
                    qt, qb_, kt_, kb_, vt, vb_ = heads[hd]
                    wT, wTb, nb = T["wT"], T["wTb"], T["nb"]
                    if T["first"]:
                        st["po"] = psO.next()
                        st["blk"] = 0
                    po_, pob = st["po"]
                    nblk_total = T["nblk"]

                    def fnO(h):
                        for j in range(nb):
                            ins = h.matmul(po_[:], wT[:, j * 128:(j + 1) * 128], vt[:, kt * 4 + j, :],
                                           start=(st["blk"] == 0), stop=(st["blk"] == nblk_total - 1))
                            st["blk"] += 1
                        return ins
                    P.op("tensor", fnO, r=[wTb, vb_], w=[pob])
                    if T["last"]:
                        P.op("scalar", lambda h: h.copy(YA[:, qb, hd * 64:(hd + 1) * 64], po_[:]), r=[pob], w=[B_YA[qb]])

                n = len(tiles)
                for s_ in range(n + 5):
                    if 0 <= s_ - 5 < n:
                        stE(tiles[s_ - 5])
                    if 0 <= s_ - 4 < n:
                        stD(tiles[s_ - 4])
                    if s_ < n:
                        stA(tiles[s_])
                    if 0 <= s_ - 1 < n:
                        stB(tiles[s_ - 1])
                    if 0 <= s_ - 2 < n:
                        stB2(tiles[s_ - 2])
                    if 0 <= s_ - 3 < n:
                        stC(tiles[s_ - 3])
            P.barrier()
            psY = Ring([ps(c, "spy%d" % i, [128, 512])[:, 0:128] for i in range(2)])
            k_ = 0
            for qb in range(NT):
                for ch in range(4):
                    py, pyb = psY.next()
                    P.op("tensor", lambda h: h.transpose(py[:], YA[:, qb, ch * 128:(ch + 1) * 128], ident[:]), r=[B_YA[qb], B_ident], w=[pyb])
                    k_ += 1
                    if k_ % 2:
                        P.op("scalar", lambda h: h.copy(yaT[:, ch, qb * 128:(qb + 1) * 128], py[:]), r=[pyb], w=[B_yaT])
                    else:
                        P.op("vector", lambda h: h.tensor_copy(yaT[:, ch, qb * 128:(qb + 1) * 128], py[:]), r=[pyb], w=[B_yaT])
            for ch in range(4):
                P.dma("sync", yT_d[ch, :, :], yaT[:, ch, :], r=[B_yaT], w=[B_yT[ch]])
        P.barrier()

    ident16 = sb(ctx, "ident16", [128, 128], BF16)
    B_id16 = Buf()
    P.op("vector", lambda h: h.tensor_copy(ident16[:], ident[:]), r=[B_ident], w=[B_id16])

    def diff_attn_phase():
        with ExitStack() as c:
            rbt = sb(c, "rbt", [128, 256])
            B_rb = Buf()
            P.dma("sync", rbt[:], rel_bias[0:1, :].to_broadcast([128, 256]), w=[B_rb])
            bk = sb(c, "bk_sb", [128, 256])
            B_bk = Buf()
            P.dma("sync", bk[:], bk_d[:, :], w=[B_bk])
            negm = sb(c, "negm_sb", [128, 128])
            B_ng = Buf()
            P.dma("sync", negm[:], negm_d[:, :], w=[B_ng])
            TB = sb(c, "TB", [128, 8, 256])
            B_TB = Buf()
            tmpr = Ring([sb(c, "tmpb%d" % i, [128, 256]) for i in range(2)])
            for hd in range(8):
                for b in range(32):
                    idx = b * 8 + hd
                    if b == 0:
                        P.op("vector", lambda h: h.tensor_scalar(TB[:, hd, :], bk[:], float(b), rbt[:, idx:idx + 1], ALU.is_equal, ALU.mult),
                             r=[B_bk, B_rb], w=[B_TB])
                    else:
                        tmpb, B_tmp = tmpr.next()
                        P.op("vector", lambda h: h.tensor_scalar(tmpb[:], bk[:], float(b), rbt[:, idx:idx + 1], ALU.is_equal, ALU.mult),
                             r=[B_bk, B_rb], w=[B_tmp])
                        P.op("vector", lambda h: h.tensor_tensor(TB[:, hd, :], TB[:, hd, :], tmpb[:], ALU.add), r=[B_tmp, B_TB], w=[B_TB])
                P.op("vector", lambda h: h.tensor_tensor(TB[:, hd, 128:256], TB[:, hd, 128:256], negm[:], ALU.add), r=[B_ng, B_TB], w=[B_TB])
            dlt = sb(c, "dlt", [128, 256])
            B_dl = Buf()
            P.dma("sync", dlt[:], dl[0:1, :].to_broadcast([128, 256]), w=[B_dl])
            lamt = sb(c, "lamt", [128, 8])
            P.op("vector", lambda h: h.tensor_tensor(dlt[:, 0:64], dlt[:, 0:64], dlt[:, 64:128], ALU.mult), r=[B_dl], w=[B_dl])
            P.op("vector", lambda h: h.tensor_tensor(dlt[:, 128:192], dlt[:, 128:192], dlt[:, 192:256], ALU.mult), r=[B_dl], w=[B_dl])
            P.op("vector", lambda h: h.tensor_reduce(lamt[:, 0:1], dlt[:, 0:64], AX.X, ALU.add), r=[B_dl], w=[B_dl])
            P.op("vector", lambda h: h.tensor_reduce(lamt[:, 1:2], dlt[:, 128:192], AX.X, ALU.add), r=[B_dl], w=[B_dl])
            P.op("scalar", lambda h: h.activation(out=lamt[:, 2:4], in_=lamt[:, 0:2], func=AF.Exp), r=[B_dl], w=[B_dl])
            P.op("vector", lambda h: h.tensor_tensor(lamt[:, 4:5], lamt[:, 3:4], lamt[:, 2:3], ALU.subtract), r=[B_dl], w=[B_dl])
            P.op("vector", lambda h: h.tensor_scalar(lamt[:, 5:6], lamt[:, 4:5], -LAMBDA_INIT, None, ALU.add), r=[B_dl], w=[B_dl])
            sgt = sb(c, "sgt", [128, 128])
            B_sg = Buf()
            P.dma("sync", sgt[:], subln[0:1, :].to_broadcast([128, 128]), w=[B_sg])
            P.op("vector", lambda h: h.tensor_scalar(sgt[:], sgt[:], 1.0 - LAMBDA_INIT, None, ALU.mult), r=[B_sg], w=[B_sg])

            ones64 = sb(c, "ones64", [64, 1], BF16)
            B_o64 = Buf()
            P.op("vector", lambda h: h.memset(ones64[:], 1.0), w=[B_o64])
            QT = [sb(c, "dq%d" % m, [64, S], BF16) for m in range(2)]
            KT = [sb(c, "dk%d" % m, [64, S], BF16) for m in range(2)]
            B_Q = bufs(2)
            B_K = bufs(2)
            sq = sb(c, "dsq", [64, S], BF16)
            B_sq = Buf()
            Vr = Ring([sb(c, "dv%d" % i, [128, NT, 128], BF16) for i in range(2)])
            Pr = Ring([sb(c, "dP%d" % i, [128, S], BF16) for i in range(4)])
            Ar = Ring([sb(c, "dA%d" % i, [128, S], BF16) for i in range(3)])
            ATr = Ring([sb(c, "dAT%d" % i, [128, NT, 128], BF16) for i in range(3)])
            str_ = Ring([sb(c, "dst%d" % i, [128, 40]) for i in range(8)])
            tsr = Ring([sb(c, "dts%d" % i, [128, 128]) for i in range(8)])
            Or = Ring([sb(c, "dO%d" % i, [128, 128]) for i in range(2)])
            jr = Ring([sb(c, "dj%d" % i, [128, 128]) for i in range(1)])
            oT = Ring([sb(c, "doT%d" % i, [128, S], BF16) for i in range(2)])
            NSH = [sb(c, "dnsh%d" % m, [128, 32]) for m in range(2)]
            NSF = [sb(c, "dnsf%d" % m, [128, 32]) for m in range(2)]
            B_NS = bufs(2)
            km = sb(c, "dkm", [128, 16])
            B_km = Buf()
            mb = sb(c, "dmb", [128, 2])
            B_mb = Buf()
            psZ = Ring([ps(c, "dpz%d" % i, [128, 512]) for i in range(3)])
            psT = Ring([ps(c, "dpt%d" % i, [128, 1024], BF16)[:, 0:512] for i in range(2)])
            psO = Ring([ps(c, "dpo%d" % i, [128, 512])[:, 0:128] for i in range(1)])
            psY = Ring([ps(c, "dpy%d" % i, [128, 512])[:, 0:128] for i in range(1)])
            psS = ps(c, "dpsS", [128, 512])
            B_psS = Buf()
            hstate = {}

            def load_head(hd):
                cfar = rbt[:, 31 * 8 + hd:31 * 8 + hd + 1]
                P.op("vector", lambda h: h.tensor_reduce(mb[:, 0:1], rbt[:, hd:256:8], AX.X, ALU.max), r=[B_rb], w=[B_mb])
                P.op("vector", lambda h: h.tensor_scalar(mb[:, 1:2], mb[:, 0:1], -1.0, None, ALU.mult), r=[B_mb], w=[B_mb])
                for m in range(2):
                    P.dma("sync", QT[m][:], qk_d[hd, m * 64:(m + 1) * 64, :], r=[B_qk[hd]], w=[B_Q[m]])
                    P.dma("scalar", KT[m][:], qk_d[8 + hd, m * 64:(m + 1) * 64, :], r=[B_qk[8 + hd]], w=[B_K[m]])
                    P.op("scalar", lambda h: h.activation(out=sq[:], in_=KT[m][:], func=AF.Square), r=[B_K[m]], w=[B_sq])
                    for t8 in range(8):
                        P.op("tensor", lambda h: h.matmul(psS[0:1, 0:512], ones64[:, 0:1], sq[:, t8 * 512:(t8 + 1) * 512], start=True, stop=True),
                             r=[B_sq, B_o64], w=[B_psS])
                        P.op("vector", lambda h: h.tensor_reduce(km[0:1, t8:t8 + 1], psS[0:1, 0:512], AX.X, ALU.max), r=[B_psS], w=[B_km])
                    P.op("vector", lambda h: h.tensor_reduce(km[0:1, 8:9], km[0:1, 0:8], AX.X, ALU.max), r=[B_km], w=[B_km])
                    P.op("tensor", lambda h: h.matmul(psS[:, 0:1], ones1[0:1, :], km[0:1, 8:9], start=True, stop=True), r=[B_km, B_ones1], w=[B_psS])
                    P.op("vector", lambda h: h.tensor_copy(km[:, 10:11], psS[:, 0:1]), r=[B_psS], w=[B_km])
                    P.op("scalar", lambda h: h.activation(out=sq[:], in_=QT[m][:], func=AF.Square), r=[B_Q[m], B_sq], w=[B_sq])

                    def fnq(h):
                        for qb in range(NT):
                            ins = h.matmul(psS[:, 32 + qb:33 + qb], sq[:, qb * 128:(qb + 1) * 128], ones64[:, 0:1], start=True, stop=True)
                        return ins
                    P.op("tensor", fnq, r=[B_sq, B_o64], w=[B_psS])
                    P.op("vector", lambda h: h.tensor_scalar(NSH[m][:], psS[:, 32:64], km[:, 10:11], None, ALU.mult), r=[B_psS, B_km], w=[B_NS[m]])
                    P.op("scalar", lambda h: h.activation(out=NSH[m][:], in_=NSH[m][:], func=AF.Sqrt), r=[B_NS[m]], w=[B_NS[m]])
                    P.op("vector", lambda h: h.tensor_scalar(NSH[m][:], NSH[m][:], -0.13, None, ALU.mult), r=[B_NS[m]], w=[B_NS[m]])
                    P.op("vector", lambda h: h.tensor_scalar(NSH[m][:], NSH[m][:], mb[:, 1:2], None, ALU.add), r=[B_NS[m], B_mb], w=[B_NS[m]])
                    P.op("vector", lambda h: h.tensor_scalar(NSF[m][:], NSH[m][:], cfar, None, ALU.add), r=[B_NS[m], B_rb], w=[B_NS[m]])
                vt, vb_ = Vr.next()
                P.dma("sync", vt[:], v_d[:, hd * 128:(hd + 1) * 128].rearrange("(b p) d -> p b d", p=128), r=B_v, w=[vb_])
                ot, otb = oT.next()
                hstate[hd] = (vt, vb_, ot, otb)

            units = [dict(hd=hd, qb=qb) for hd in range(8) for qb in range(NT)]
            cnt = {"k": 0}

            def stA(U):
                hd, qb = U["hd"], U["qb"]
                if qb == 0:
                    load_head(hd)
                L = (qb + 1) * 128
                nkt = (L + 511) // 512
                st, stb = str_.next()
                P.op("vector", lambda h: h.memset(st[:], 0.0), w=[stb])
                Pm = []
                for m in range(2):
                    Pt, Ptb = Pr.next()
                    for kt in range(nkt):
                        W = min(512, L - kt * 512)
                        pz, pzb = psZ.next()
                        P.op("tensor", lambda h: h.matmul(pz[:, 0:W], QT[m][:, qb * 128:(qb + 1) * 128], KT[m][:, kt * 512:kt * 512 + W],
                                                          start=True, stop=True), r=[B_Q[m], B_K[m]], w=[pzb])
                        c_lo = kt * 512
                        c_hi = c_lo + W
                        far_hi = min(c_hi, max(c_lo, L - 256))
                        near = []
                        for (b_lo, tb_lo, col) in ((L - 256, 0, 8), (L - 128, 128, 9)):
                            if b_lo >= c_lo and b_lo < c_hi and b_lo >= 0:
                                o_ = b_lo - c_lo
                                ts, tsb = tsr.next()
                                P.op("vector", lambda h: h.scalar_tensor_tensor(ts[:], pz[:, o_:o_ + 128], 0.125, TB[:, hd, tb_lo:tb_lo + 128],
                                                                               ALU.mult, ALU.add), r=[pzb, B_TB], w=[tsb])
                                near.append((b_lo, col, ts, tsb))
                        if far_hi > c_lo:
                            n_ = far_hi - c_lo
                            P.op("scalar", lambda h: h.activation(out=Pt[:, c_lo:far_hi], in_=pz[:, 0:n_], func=AF.Exp, scale=0.125,
                                                                  bias=NSF[m][:, qb:qb + 1], accum_out=st[:, m * 10 + kt:m * 10 + kt + 1]),
                                 r=[pzb, B_NS[m]] + [x[3] for x in near], w=[Ptb, stb])
                        for (b_lo, col, ts, tsb) in near:
                            P.op("scalar", lambda h: h.activation(out=Pt[:, b_lo:b_lo + 128], in_=ts[:], func=AF.Exp,
                                                                  bias=NSH[m][:, qb:qb + 1], accum_out=st[:, m * 10 + col:m * 10 + col + 1]),
                                 r=[tsb, B_NS[m]], w=[Ptb, stb])
                    Pm.append((Pt, Ptb))
                U["st"], U["stb"], U["Pm"], U["L"] = st, stb, Pm, L

            def stB(U):
                st, stb, Pm, L = U["st"], U["stb"], U["Pm"], U["L"]
                P.op("vector", lambda h: h.tensor_reduce(st[:, 20:21], st[:, 0:10], AX.X, ALU.add), r=[stb], w=[stb])
                P.op("vector", lambda h: h.tensor_reduce(st[:, 21:22], st[:, 10:20], AX.X, ALU.add), r=[stb], w=[stb])
                P.op("vector", lambda h: h.reciprocal(st[:, 22:24], st[:, 20:22]), r=[stb], w=[stb])
                P.op("vector", lambda h: h.tensor_tensor(st[:, 24:25], st[:, 23:24], lamt[:, 5:6], ALU.mult), r=[stb, B_dl], w=[stb])
                At, Atb = Ar.next()
                P.op("vector", lambda h: h.tensor_scalar(At[:, 0:L], Pm[0][0][:, 0:L], st[:, 22:23], None, ALU.mult),
                     r=[Pm[0][1], stb], w=[Atb])
                P.op("vector", lambda h: h.scalar_tensor_tensor(At[:, 0:L], Pm[1][0][:, 0:L], st[:, 24:25], At[:, 0:L], ALU.mult, ALU.add),
                     r=[Pm[1][1], stb, Atb], w=[Atb])
                U["At"], U["Atb"] = At, Atb

            def stC(U):
                At, Atb, L = U["At"], U["Atb"], U["L"]
                ATt, ATb = ATr.next()
                nblk = L // 128
                for g4 in range((nblk + 3) // 4):
                    nb = min(4, nblk - g4 * 4)
                    pt_, ptb = psT.next()

                    def fnT(h):
                        for j in range(nb):
                            cb = (g4 * 4 + j) * 128
                            ins = h.transpose(pt_[:, j * 128:(j + 1) * 128], At[:, cb:cb + 128], ident16[:])
                        return ins
                    P.op("tensor", fnT, r=[Atb, B_id16], w=[ptb])
                    dstv = ATt[:, g4 * 4:g4 * 4 + nb, :]
                    srcv = pt_[:, 0:nb * 128].rearrange("p (j t) -> p j t", j=nb)
                    cnt["k"] += 1
                    if cnt["k"] % 2 == 0:
                        P.op("scalar", lambda h: h.copy(dstv, srcv), r=[ptb], w=[ATb])
                    else:
                        P.op("vector", lambda h: h.tensor_copy(dstv, srcv), r=[ptb], w=[ATb])
                U["ATt"], U["ATb"] = ATt, ATb

            def stD(U):
                hd, qb, L = U["hd"], U["qb"], U["L"]
                st, stb = U["st"], U["stb"]
                ATt, ATb = U["ATt"], U["ATb"]
                vt, vb_, ot, otb = hstate[hd]
                nblk = L // 128
                po_, pob = psO.next()

                def fnO(h):
                    for j in range(nblk):
                        ins = h.matmul(po_[:], ATt[:, j, :], vt[:, j, :], start=(j == 0), stop=(j == nblk - 1))
                    return ins
                P.op("tensor", fnO, r=[ATb, vb_], w=[pob])
                jt, jb = jr.next()
                P.op("scalar", lambda h: h.activation(out=jt[:], in_=po_[:], func=AF.Square, accum_out=st[:, 30:31]), r=[pob], w=[jb, stb])
                P.op("scalar", lambda h: h.activation(out=st[:, 31:32], in_=st[:, 30:31], func=AF.Ln, scale=1.0 / 128, bias=EPS_AP[:, 0:1]),
                     r=[stb, B_eps], w=[stb])
                P.op("scalar", lambda h: h.activation(out=st[:, 32:33], in_=st[:, 31:32], func=AF.Exp, scale=-0.5), r=[stb], w=[stb])
                Ot, Otb = Or.next()
                P.op("vector", lambda h: h.scalar_tensor_tensor(Ot[:], po_[:], st[:, 32:33], sgt[:], ALU.mult, ALU.mult),
                     r=[pob, stb, B_sg], w=[Otb])
                py, pyb = psY.next()
                P.op("tensor", lambda h: h.transpose(py[:], Ot[:], ident[:]), r=[Otb, B_ident], w=[pyb])
                P.op("scalar", lambda h: h.copy(ot[:, qb * 128:(qb + 1) * 128], py[:]), r=[pyb], w=[otb])
                if qb == NT - 1:
                    P.dma("sync", yT_d[hd, :, :], ot[:], r=[otb], w=[B_yT[hd]])

            n = len(units)
            for s_ in range(n + 3):
                if 0 <= s_ - 3 < n:
                    stD(units[s_ - 3])
                if s_ < n:
                    stA(units[s_])
                if 0 <= s_ - 1 < n:
                    stB(units[s_ - 1])
                if 0 <= s_ - 2 < n:
                    stC(units[s_ - 2])
        P.barrier()

    L0, L1 = 0, 6144
    if upto >= 1 and lo_ph <= 1:
        proj_phase("p0", x_in, None, even_w_in, L0 + 1024, L0 + 0, [
            (0, 1024, "F", qk_d, B_qk, BF16, 0),
            (1024, 512, "T", v_d, B_v, BF16, 0),
            (1536, 1024, "F", xg_d, B_xg, F32, 0),
        ])
    if upto >= 2 and lo_ph <= 2:
        lru_phase()
    if upto >= 3 and lo_ph <= 3:
        sb_attn_phase()
    if upto >= 4 and lo_ph <= 4:
        outproj_phase("o0", x_in, None, xs_d[0], B_xs[0], even_w_out, L0 + 2048)
    if upto >= 5 and lo_ph <= 5:
        ffn_phase("f0", xs_d[0], B_xs[0], xs_d[1], B_xs[1], ffn_wg, ffn_wu, ffn_wd, 1, 2816, L0 + 4096, L0 + 3072, L0 + 5120)
    if upto >= 6 and lo_ph <= 6:
        proj_phase("p1", xs_d[1], B_xs[1], odd_w_in, L1 + 1024, L1 + 0, [
            (0, 2048, "F", qk_d, B_qk, BF16, 0),
            (2048, 1024, "T", v_d, B_v, BF16, 0),
        ])
    if upto >= 7 and lo_ph <= 7:
        diff_attn_phase()
    if upto >= 8 and lo_ph <= 8:
        outproj_phase("o1", xs_d[1], B_xs[1], xs_d[2], B_xs[2], odd_w_out, L1 + 2048)
    if upto >= 9:
        ffn_phase("f1", xs_d[2], B_xs[2], out_d, B_out, moe_wg, moe_wu, moe_wd, 8, 3584, L1 + 4096, L1 + 3072, L1 + 5120, final=True)
    else:
        zt = sb(ctx, "zt", [128, D])
        bz = Buf()
        P.op("vector", lambda h: h.memset(zt[:], 0.0), w=[bz])
        P.dma("sync", out_d[0:128, :], zt[:], r=[bz], w=[B_out[0]])
    P.barrier()
    ctx.close()
    return nc, P


def _bucket_table():
    n = np.arange(0, 256, dtype=np.int64)
    nf = np.maximum(n, 1).astype(np.float32)
    large = 16 + (np.log(nf / np.float32(16)) / np.float32(math.log(128 / 16)) * np.float32(16)).astype(np.int32)
    large = np.minimum(large, 31)
    return np.where(n < 16, n, large)


def make_in_maps(inp, names=None):
    f = lambda a: np.ascontiguousarray(np.asarray(a, dtype=np.float32))
    tq = np.arange(128)[:, None]
    sk = np.arange(128)[None, :]
    bt = _bucket_table()
    bk = np.zeros((128, 256), np.float32)
    bk[:, 0:128] = bt[128 + tq - sk]
    rel0 = tq - sk
    bk[:, 128:256] = np.where(rel0 >= 0, bt[np.maximum(rel0, 0)], -1)
    lru_vec = np.zeros((128, 4, 8), np.float32)
    cw = f(inp["lru_conv_w"])[0]
    for i in range(4):
        lru_vec[:, :, i] = cw[i].reshape(4, 128).T
    for j, kname in enumerate(("lru_conv_b", "lru_gate_a_b", "lru_gate_x_b", "lru_lambda")):
        lru_vec[:, :, 4 + j] = f(inp[kname])[0].reshape(4, 128).T
    shared = {
        "rel_bias": f(inp["rel_bias"]).reshape(1, 256),
        "ada_w": f(inp["ada_w"]),
        "ada_b": f(inp["ada_b"]).reshape(1, 12 * D),
        "ln_all": np.concatenate([f(inp["ln_mix"])[0], f(inp["ln_ffn"])[0], f(inp["ln_mix"])[1], f(inp["ln_ffn"])[1],
                                  f(inp["ln_final"])]).reshape(1, 5 * D),
        "even_w_in": f(inp["even_w_in"])[0],
        "even_w_out": f(inp["even_w_out"])[0],
        "lru_vec": lru_vec,
        "lru_ga": f(inp["lru_gate_a_w"])[0],
        "lru_gx": f(inp["lru_gate_x_w"])[0],
        "ffn_wg": f(inp["ffn_w_gate"]),
        "ffn_wu": f(inp["ffn_w_up"]),
        "ffn_wd": f(inp["ffn_w_down"]),
        "odd_w_in": f(inp["odd_w_in"])[0],
        "odd_w_out": f(inp["odd_w_out"])[0],
        "dl": np.concatenate([f(inp["diff_lambda_q1"])[0], f(inp["diff_lambda_k1"])[0], f(inp["diff_lambda_q2"])[0],
                              f(inp["diff_lambda_k2"])[0]]).reshape(1, 256),
        "subln": f(inp["diff_subln"]).reshape(1, 128),
        "router_w": f(inp["router_w"])[0],
        "router_b": f(inp["router_b"]).reshape(1, 8),
        "moe_wg": f(inp["moe_w_gate"])[0],
        "moe_wu": f(inp["moe_w_up"])[0],
        "moe_wd": f(inp["moe_w_down"])[0],
        "ident": np.eye(128, dtype=np.float32),
        "lmask": (sk < tq).astype(np.float32),
        "bk": bk,
        "negm": np.where(sk > tq, np.float32(-1e30), np.float32(0)).astype(np.float32),
    }
    x = f(inp["x"])
    cc = f(inp["c"])
    maps = []
    for b in range(8):
        m = dict(shared)
        m["x"] = x[b]
        m["cT"] = np.ascontiguousarray(cc[b].reshape(8, 128).T)
        if names is not None:
            m = {k: v for k, v in m.items() if k in names}
        maps.append(m)
    return maps


def kernel(**inputs):
    nc, _ = build()
    maps = make_in_maps(inputs)
    res = run_bass_kernel_spmd(nc, maps, core_ids=list(range(8)))
    return np.stack([np.asarray(r["out"], dtype=np.float32) for r in res.results], axis=0)
```
